# Optimizing a Trainium2 kernel written in Bass

```python
import math
import jax
import jax.numpy as jnp
from jax import lax
import numpy as np

D_MODEL = 1024
BATCH = 32
SEQ = 2048
DEPTH = 4

MIX_WIDTH = D_MODEL
ATTN_HEAD_DIM = 64
ATTN_WIDTH = MIX_WIDTH // 4
ATTN_HEADS = ATTN_WIDTH // ATTN_HEAD_DIM
DILATED_PATTERNS = ((128, 1), (512, 4), (2048, 16))
ROPE_THETA = 10000.0
CONV_CH = MIX_WIDTH // 4
CONV_WIDTH = 31
GDN_HEAD_DIM = 128
GDN_WIDTH = MIX_WIDTH - ATTN_WIDTH - CONV_CH
GDN_HEADS = GDN_WIDTH // GDN_HEAD_DIM
GDN_CONV_WIDTH = 4
GDN_CHUNK = 64
FFN_HIDDEN = ((8 * D_MODEL + 3 * 256 - 1) // (3 * 256)) * 256
IN_SPLIT_SIZES = (ATTN_WIDTH, ATTN_WIDTH, ATTN_WIDTH, 2 * CONV_CH, 3 * GDN_WIDTH, GDN_WIDTH, GDN_HEADS, GDN_HEADS)
IN_WIDTH = sum(IN_SPLIT_SIZES)
RMS_EPS = 1e-6
LN_EPS = 1e-5
L2_EPS = 1e-6

kernel_name = "hybrid_dilated_conformer_gdn_trunk"


def rms_norm(x, gain):
    x32 = x.astype(jnp.float32)
    y = x32 * lax.rsqrt(jnp.mean(x32 * x32, axis=-1, keepdims=True) + RMS_EPS)
    return (y * gain.astype(jnp.float32)).astype(x.dtype)


def l2_normalize(t):
    return t * lax.rsqrt(jnp.sum(t * t, axis=-1, keepdims=True) + L2_EPS)


def causal_depthwise_conv(x, w):
    K, C = w.shape
    return lax.conv_general_dilated(
        x, w.reshape(K, 1, C).astype(x.dtype), window_strides=(1,), padding=[(K - 1, 0)],
        dimension_numbers=('NWC', 'WIO', 'NWC'), feature_group_count=C)


def rotary(t, positions):
    D = t.shape[-1]
    half = D // 2
    inv_freq = jnp.exp(-math.log(ROPE_THETA) * jnp.arange(half, dtype=jnp.float32) * (2.0 / D))
    ang = positions.astype(jnp.float32)[:, None] * inv_freq[None, :]
    cos = jnp.cos(ang)[None, :, None, :]
    sin = jnp.sin(ang)[None, :, None, :]
    t1, t2 = t[..., :half], t[..., half:]
    return jnp.concatenate([t1 * cos - t2 * sin, t2 * cos + t1 * sin], axis=-1)


def dilated_window_attention(q, k, v, window, dilation):
    Bn, S, H, D = q.shape
    n = window // dilation
    L = S // dilation
    nb = -(-L // n)
    Lp = nb * n

    def residues(t):
        t = t.reshape(Bn, L, dilation, H, D)
        return jnp.pad(t, ((0, 0), (0, Lp - L), (0, 0), (0, 0), (0, 0)))

    def key_blocks(t):
        tp = jnp.pad(residues(t), ((0, 0), (n, 0), (0, 0), (0, 0), (0, 0)))
        tp = tp.reshape(Bn, nb + 1, n, dilation, H, D)
        return jnp.concatenate([tp[:, :-1], tp[:, 1:]], axis=2)

    qb = residues(q).reshape(Bn, nb, n, dilation, H, D)
    kb = key_blocks(k)
    vb = key_blocks(v)
    s = jnp.einsum('bnqrhd,bnkrhd->bnrhqk', qb, kb)
    qi = jnp.arange(n)[None, :, None]
    ki = jnp.arange(2 * n)[None, None, :]
    blk = jnp.arange(nb)[:, None, None]
    valid = (ki >= qi) & (ki <= qi + n) & (blk * n - n + ki >= 0)
    s = jnp.where(valid[None, :, None, None], s, -jnp.inf)
    m = jnp.max(s, axis=-1, keepdims=True)
    p = jnp.exp(s - m)
    l = jnp.sum(p, axis=-1, keepdims=True)
    o = jnp.einsum('bnrhqk,bnkrhd->bnqrhd', p / l, vb)
    o = o.reshape(Bn, Lp, dilation, H, D)[:, :L].reshape(Bn, S, H, D)
    lse = (m + jnp.log(l))[..., 0]
    lse = lse.transpose(0, 1, 4, 2, 3).reshape(Bn, Lp, dilation, H)[:, :L].reshape(Bn, S, H)
    return o, lse


def dilated_attention_mixer(aq, ak, av, positions):
    Bn, S, _ = aq.shape
    shp = (Bn, S, ATTN_HEADS, ATTN_HEAD_DIM)
    q = rotary(aq.reshape(shp).astype(jnp.float32), positions) * (ATTN_HEAD_DIM ** -0.5)
    k = rotary(ak.reshape(shp).astype(jnp.float32), positions)
    v = av.reshape(shp).astype(jnp.float32)
    outs, lses = [], []
    for window, dilation in DILATED_PATTERNS:
        o, lse = dilated_window_attention(q, k, v, window, dilation)
        outs.append(o)
        lses.append(lse)
    wts = jax.nn.softmax(jnp.stack(lses, axis=0), axis=0)
    o = jnp.einsum('pbsh,pbshd->bshd', wts, jnp.stack(outs, axis=0))
    return o.reshape(Bn, S, ATTN_WIDTH)


def conformer_conv_mixer(u, dw, dw_bias, norm_gain, norm_bias):
    val, gate = jnp.split(u, 2, axis=-1)
    y = val * jax.nn.sigmoid(gate)
    y = causal_depthwise_conv(y, dw) + dw_bias.astype(y.dtype)
    y32 = y.astype(jnp.float32)
    mu = jnp.mean(y32, axis=-1, keepdims=True)
    var = jnp.mean(jnp.square(y32 - mu), axis=-1, keepdims=True)
    y32 = (y32 - mu) * lax.rsqrt(var + LN_EPS) * norm_gain.astype(jnp.float32) + norm_bias.astype(jnp.float32)
    return jax.nn.silu(y32)


def chunk_gated_delta_rule(q, k, v, g, beta):
    Bn, S, H, Dk = q.shape
    Dv = v.shape[-1]
    C = GDN_CHUNK
    N = S // C
    q = q * (Dk ** -0.5)

    def to_chunks(t):
        return t.reshape(Bn, N, C, H, -1).transpose(0, 3, 1, 2, 4)

    q, k, v = to_chunks(q), to_chunks(k), to_chunks(v)
    g = jnp.cumsum(g.reshape(Bn, N, C, H).transpose(0, 3, 1, 2), axis=-1)
    beta = beta.reshape(Bn, N, C, H).transpose(0, 3, 1, 2)
    idx = jnp.arange(C)
    lower_incl = idx[:, None] >= idx[None, :]
    strict = idx[:, None] > idx[None, :]
    decay = jnp.exp(jnp.where(lower_incl, g[..., :, None] - g[..., None, :], -jnp.inf))
    k_beta = k * beta[..., None]
    v_beta = v * beta[..., None]
    m = jnp.where(strict, jnp.einsum('bhncd,bhnsd->bhncs', k_beta, k) * decay, 0.0)
    rhs = jnp.concatenate([v_beta, k_beta * jnp.exp(g)[..., None]], axis=-1)
    sol = lax.linalg.triangular_solve(m + jnp.eye(C, dtype=jnp.float32), rhs, left_side=True, lower=True,
                                      unit_diagonal=True)
    u = sol[..., :Dv]
    w = sol[..., Dv:]
    intra = jnp.where(lower_incl, jnp.einsum('bhncd,bhnsd->bhncs', q, k) * decay, 0.0)

    def step(state, xs):
        q_c, k_c, u_c, w_c, g_c, intra_c = xs
        v_new = u_c - w_c @ state
        out = (q_c * jnp.exp(g_c)[..., None]) @ state + intra_c @ v_new
        g_last = g_c[..., -1]
        state = state * jnp.exp(g_last)[..., None, None] + jnp.einsum(
            'bhcd,bhce->bhde', k_c * jnp.exp(g_last[..., None] - g_c)[..., None], v_new)
        return state, out

    xs = tuple(jnp.moveaxis(t, 2, 0) for t in (q, k, u, w, g, intra))
    state0 = jnp.zeros((Bn, H, Dk, Dv), jnp.float32)
    _, out = lax.scan(step, state0, xs)
    return out.transpose(1, 0, 3, 2, 4).reshape(Bn, S, H, Dv)


def gated_deltanet_mixer(qkv, gate, a, b, short_conv, a_log, dt_bias, out_norm):
    Bn, S, _ = qkv.shape
    qkv = jax.nn.silu(causal_depthwise_conv(qkv, short_conv).astype(jnp.float32))
    shp = (Bn, S, GDN_HEADS, GDN_HEAD_DIM)
    q, k, v = (t.reshape(shp) for t in jnp.split(qkv, 3, axis=-1))
    q = l2_normalize(q)
    k = l2_normalize(k)
    beta = jax.nn.sigmoid(b.astype(jnp.float32))
    g = -jnp.exp(a_log.astype(jnp.float32)) * jax.nn.softplus(a.astype(jnp.float32) + dt_bias.astype(jnp.float32))
    o = chunk_gated_delta_rule(q, k, v, g, beta)
    o = rms_norm(o, out_norm) * jax.nn.silu(gate.astype(jnp.float32).reshape(shp))
    return o.reshape(Bn, S, GDN_WIDTH)


def setup_inputs(seed: int = 0) -> dict:
    key = jax.random.key(seed)
    ks = jax.random.split(key, 20)
    f32 = jnp.float32

    def nrm(k, shape, fan_in):
        return jax.random.normal(k, shape, f32) * (fan_in ** -0.5)

    def gain(k, shape):
        return 1.0 + 0.01 * jax.random.normal(k, shape, f32)

    dt = jnp.exp(jax.random.uniform(ks[10], (DEPTH, GDN_HEADS), f32, math.log(1e-3), math.log(1e-1)))
    return {
        'x': jax.random.normal(ks[0], (BATCH, SEQ, D_MODEL), f32),
        'attn_pre_norm': gain(ks[1], (DEPTH, D_MODEL)),
        'w_in': nrm(ks[2], (DEPTH, D_MODEL, IN_WIDTH), D_MODEL),
        'conv_dw': nrm(ks[3], (DEPTH, CONV_WIDTH, CONV_CH), CONV_WIDTH),
        'conv_dw_bias': 0.01 * jax.random.normal(ks[4], (DEPTH, CONV_CH), f32),
        'conv_norm_gain': gain(ks[5], (DEPTH, CONV_CH)),
        'conv_norm_bias': 0.01 * jax.random.normal(ks[6], (DEPTH, CONV_CH), f32),
        'gdn_short_conv': nrm(ks[7], (DEPTH, GDN_CONV_WIDTH, 3 * GDN_WIDTH), GDN_CONV_WIDTH),
        'gdn_a_log': jnp.log(jax.random.uniform(ks[8], (DEPTH, GDN_HEADS), f32, 1.0, 16.0)),
        'gdn_dt_bias': dt + jnp.log(-jnp.expm1(-dt)),
        'gdn_out_norm': gain(ks[9], (DEPTH, GDN_HEAD_DIM)),
        'w_out': nrm(ks[11], (DEPTH, MIX_WIDTH, D_MODEL), MIX_WIDTH),
        'attn_post_norm': gain(ks[12], (DEPTH, D_MODEL)),
        'ffn_pre_norm': gain(ks[13], (DEPTH, D_MODEL)),
        'w_gate': nrm(ks[14], (DEPTH, D_MODEL, FFN_HIDDEN), D_MODEL),
        'w_up': nrm(ks[15], (DEPTH, D_MODEL, FFN_HIDDEN), D_MODEL),
        'w_down': nrm(ks[16], (DEPTH, FFN_HIDDEN, D_MODEL), FFN_HIDDEN),
        'ffn_post_norm': gain(ks[17], (DEPTH, D_MODEL)),
    }


def reference(x, attn_pre_norm, w_in, conv_dw, conv_dw_bias, conv_norm_gain, conv_norm_bias, gdn_short_conv,
              gdn_a_log, gdn_dt_bias, gdn_out_norm, w_out, attn_post_norm, ffn_pre_norm, w_gate, w_up, w_down,
              ffn_post_norm):
    Bn, S, _ = x.shape
    positions = jnp.arange(S)
    offsets = np.cumsum(IN_SPLIT_SIZES)[:-1].tolist()
    for i in range(DEPTH):
        h = rms_norm(x, attn_pre_norm[i])
        z = h @ w_in[i]
        aq, ak, av, conv_u, gdn_qkv, gdn_gate, gdn_a, gdn_b = jnp.split(z, offsets, axis=-1)
        y_attn = dilated_attention_mixer(aq, ak, av, positions)
        y_conv = conformer_conv_mixer(conv_u, conv_dw[i], conv_dw_bias[i], conv_norm_gain[i], conv_norm_bias[i])
        y_gdn = gated_deltanet_mixer(gdn_qkv, gdn_gate, gdn_a, gdn_b, gdn_short_conv[i], gdn_a_log[i],
                                     gdn_dt_bias[i], gdn_out_norm[i])
        mix = jnp.concatenate([y_attn.astype(x.dtype), y_conv.astype(x.dtype), y_gdn.astype(x.dtype)], axis=-1)
        x = x + rms_norm(mix @ w_out[i], attn_post_norm[i])
        h = rms_norm(x, ffn_pre_norm[i])
        f = (jax.nn.silu(h @ w_gate[i]) * (h @ w_up[i])) @ w_down[i]
        x = x + rms_norm(f, ffn_post_norm[i])
    return x
```

```python
import contextlib
import math
import numpy as np
import concourse.bass as bass
import concourse.mybir as mybir
from concourse.bass_utils import run_bass_kernel_spmd

F32 = mybir.dt.float32
BF16 = mybir.dt.bfloat16
AF = mybir.ActivationFunctionType
ALU = mybir.AluOpType

D = 1024
S = 2048
L = 4
NCORE = 8
SEQ_PER_CORE = 4
FF = 2816
KC = 8
FC = 22
NWIN = 31
RMS_EPS = 1e-6
LN_EPS = 1e-5
L2_EPS = 1e-6
NEG = -30000.0
MS_OFF = 384
MS_W = 2048 + MS_OFF
PP_PER_L = 149
ENGS = ("pe", "act", "dve", "pool", "sp")


class Res:
    __slots__ = ("name", "w", "r")

    def __init__(self, name):
        self.name = name
        self.w = None
        self.r = {}


class Op:
    __slots__ = ("eng", "fn", "waits", "sig", "sigval", "dma", "idx")


class Prog:
    def __init__(self, nc):
        self.nc = nc
        self.ops = {e: [] for e in ENGS}
        self.seen = {e: {} for e in ENGS}
        self.dma_sems = {}

    def _need(self, op, ev):
        if ev is None:
            return
        kind, k, val = ev
        if kind == "e" and k == "pe" and op.eng == "pe":
            return
        key = (kind, k)
        if self.seen[op.eng].get(key, -1) >= val:
            return
        if val > op.waits.get(key, -1):
            op.waits[key] = val

    def op(self, eng, fn, reads=(), writes=(), dma_key=None):
        o = Op()
        o.eng = eng
        o.fn = fn
        o.waits = {}
        o.sig = False
        o.sigval = None
        o.dma = dma_key
        o.idx = len(self.ops[eng])
        for r in reads:
            self._need(o, r.w)
        for w in writes:
            self._need(o, w.w)
            for k2, v2 in w.r.items():
                self._need(o, (k2[0], k2[1], v2))
        for key, v in o.waits.items():
            self.seen[eng][key] = v
            if key[0] == "e":
                self.ops[key[1]][v].sig = True
        if dma_key is not None:
            ent = self.dma_sems.setdefault(dma_key, [None, 0])
            ent[1] += 16
            ev = ("d", dma_key, ent[1])
        else:
            ev = ("e", eng, o.idx)
        rk = (ev[0], ev[1])
        for r in reads:
            if r.r.get(rk, -1) < ev[2]:
                r.r[rk] = ev[2]
        for w in writes:
            w.w = ev
            w.r = {}
        self.ops[eng].append(o)
        return o

    def barrier(self, engines=("pe", "act", "dve", "sp")):
        last = {}
        for e in engines:
            i = len(self.ops[e]) - 1
            while i >= 0 and self.ops[e][i].dma is not None:
                i -= 1
            last[e] = i
        dm = {k: v[1] for k, v in self.dma_sems.items()}
        for e in engines:
            o = Op()
            o.eng = e
            o.fn = lambda eng: eng.nop()
            o.waits = {}
            o.sig = False
            o.sigval = None
            o.dma = None
            o.idx = len(self.ops[e])
            for b in engines:
                if last[b] >= 0:
                    self._need(o, ("e", b, last[b]))
            for k, v in dm.items():
                self._need(o, ("d", k, v))
            for key, v in o.waits.items():
                self.seen[e][key] = v
                if key[0] == "e":
                    self.ops[key[1]][v].sig = True
            self.ops[e].append(o)

    def emit(self, final_waits=()):
        nc = self.nc
        with contextlib.ExitStack() as st:
            esem = {e: st.enter_context(nc.semaphore("s_" + e)) for e in ENGS}
            for i, (k, ent) in enumerate(self.dma_sems.items()):
                ent[0] = st.enter_context(nc.semaphore("d%d" % i))
            for e in ENGS:
                c = 0
                for o in self.ops[e]:
                    if o.sig and o.dma is None:
                        c += 1
                        o.sigval = c
            block = st.enter_context(nc.Block())

            def run(ename):
                def body(eng):
                    for o in self.ops[ename]:
                        ws = []
                        for key, v in o.waits.items():
                            if key[0] == "e":
                                ws.append((esem[key[1]], self.ops[key[1]][v].sigval))
                            else:
                                ws.append((self.dma_sems[key[1]][0], v))
                        for (s, v) in ws:
                            eng.wait_ge(s, v)
                        ins = o.fn(eng)
                        if o.dma is not None:
                            ins.then_inc(self.dma_sems[o.dma][0], 16)
                        elif o.sig:
                            ins.then_inc(esem[ename], 1)
                    if ename == "sp":
                        for k in final_waits:
                            eng.wait_ge(self.dma_sems[k][0], self.dma_sems[k][1])
                return body

            block.tensor(run("pe"))
            block.scalar(run("act"))
            block.vector(run("dve"))
            block.gpsimd(run("pool"))
            block.sync(run("sp"))


def _win_cols():
    A = 256
    cols = []
    q0, k0, v0 = 0, A, 2 * A

    def swap64(base):
        idx = np.arange(128)
        hd = idx // 64
        d = idx % 64
        return base + hd * 64 + (d + 32) % 64

    for base in (q0, q0 + 128, k0, k0 + 128):
        cols.append(base + np.arange(128))
        cols.append(swap64(base))
    cols.append(v0 + np.arange(128))
    cols.append(v0 + 128 + np.arange(128))
    cu = 3 * A
    cols.append(cu + np.arange(128))
    cols.append(cu + 256 + np.arange(128))
    cols.append(cu + 128 + np.arange(128))
    cols.append(cu + 384 + np.arange(128))
    gq = 3 * A + 512
    gg = gq + 1536
    ga = gg + 512
    cols.append(np.concatenate([ga + np.arange(8), np.full(120, -1)]))
    for h in range(4):
        cols.append(gq + h * 128 + np.arange(128))
        cols.append(gq + 512 + h * 128 + np.arange(128))
        cols.append(gq + 1024 + h * 128 + np.arange(128))
        cols.append(gg + h * 128 + np.arange(128))
    return cols


def _chunk_w(w, ncols_list):
    K = w.shape[0]
    out = np.zeros((len(ncols_list), 128, K // 128, 128), np.float32)
    for i, cl in enumerate(ncols_list):
        cl = np.asarray(cl)
        ok = cl >= 0
        blk = np.zeros((K, 128), np.float32)
        blk[:, ok] = w[:, cl[ok]]
        out[i] = blk.reshape(K // 128, 128, 128).transpose(1, 0, 2)
    return out


def _host_consts():
    pos = np.arange(S, dtype=np.float32)
    half = 32
    inv_freq = np.exp(-math.log(10000.0) * np.arange(half, dtype=np.float32) * (2.0 / 64)).astype(np.float32)
    ang = pos[None, :] * inv_freq[:, None]
    cos = np.cos(ang).astype(np.float32)
    sin = np.sin(ang).astype(np.float32)
    d = np.arange(128) % 64
    cs = np.zeros((2, 128, S), np.float32)
    cs[0] = cos[d % 32]
    cs[1] = np.where((d < 32)[:, None], -sin[d % 32], sin[d % 32])
    delta = np.arange(MS_W)[None, :] - MS_OFF - np.arange(128)[:, None]
    c = ((delta >= 0) & (delta <= 128)).astype(np.float32)
    c += ((delta >= 0) & (delta <= 512) & (delta % 4 == 0))
    c += ((delta >= 0) & (delta <= 2048) & (delta % 16 == 0))
    ms = c.astype(np.float32)
    p = np.arange(128)[:, None]
    f = np.arange(128)[None, :]
    ident = (p == f).astype(np.float32)
    tri = (p <= f).astype(np.float32)
    nmls = np.where(p <= f, NEG, 0.0).astype(np.float32)
    nmui = np.where(f < p, NEG, 0.0).astype(np.float32)
    cmat = np.concatenate([ident, tri, nmls, nmui], axis=1).astype(np.float32)
    return cs, ms, cmat


class Builder:
    def __init__(self, nseq, nlayers, taps=(), stop=None):
        self.stop = stop
        self.nseq = nseq
        self.nl = nlayers
        self.taps = set(taps)
        self.tap_out = {}
        nc = bass.Bass("TRN2", target_bir_lowering=False)
        self.nc = nc
        dt = nc.dram_tensor
        self.d_x = dt("xT", [nseq, D, S], F32, kind="ExternalInput").ap()
        self.d_win = dt("win", [L, NWIN, 128, KC * 128], F32, kind="ExternalInput").ap()
        self.d_wout = dt("wout", [L, KC, 128, KC * 128], F32, kind="ExternalInput").ap()
        self.d_wg = dt("wgate", [L, FC, 128, KC * 128], F32, kind="ExternalInput").ap()
        self.d_wu = dt("wup", [L, FC, 128, KC * 128], F32, kind="ExternalInput").ap()
        self.d_wd = dt("wdown", [L, KC, 128, FC * 128], F32, kind="ExternalInput").ap()
        self.d_pp = dt("pp", [128, L * PP_PER_L], F32, kind="ExternalInput").ap()
        self.d_gdnc = dt("gdnc", [1, L * 128], F32, kind="ExternalInput").ap()
        self.d_cs = dt("cs", [2, 128, S], F32, kind="ExternalInput").ap()
        self.d_ms = dt("ms", [128, MS_W], F32, kind="ExternalInput").ap()
        self.d_cmat = dt("cmat", [128, 512], F32, kind="ExternalInput").ap()
        self.d_out = dt("outT", [nseq, D, S], F32, kind="ExternalOutput").ap()
        self.P = Prog(nc)
        self.RA = 6
        self.RB = 2

    def alloc(self, st):
        nc = self.nc

        def sb(name, shape, dtp):
            return st.enter_context(nc.sbuf_tensor(name, shape, dtp))

        self.xT = sb("xT_sb", [128, KC, S], F32)
        self.rX = [[Res("x%d_%d" % (c, t)) for t in range(4)] for c in range(KC)]
        self.cmat = sb("cmat_sb", [128, 512], F32)
        self.ident_f = self.cmat[:, 0:128]
        self.tri_f = self.cmat[:, 128:256]
        self.nmls = self.cmat[:, 256:384]
        self.nmui = self.cmat[:, 384:512]
        self.ident_b = sb("ident_b", [128, 128], BF16)
        self.ones_b = sb("ones_b", [128, 4, 128], BF16)
        self.ones_f = sb("ones_f", [128, 128], F32)
        self.pp = sb("pp_sb", [128, L * PP_PER_L], F32)
        self.gdnc = sb("gdnc_sb", [128, L * 128], F32)
        self.rC = Res("consts")
        self.ringA = [sb("ra%d" % i, [128, KC, 128], BF16) for i in range(self.RA)]
        self.rA = [Res("ra%d" % i) for i in range(self.RA)]
        self.ringB = [sb("rb%d" % i, [128, FC, 128], BF16) for i in range(self.RB)]
        self.rB = [Res("rb%d" % i) for i in range(self.RB)]
        self.ARW = 28672
        self.arena = sb("arena", [128, self.ARW], F32)
        self.banks = [st.enter_context(nc.psum_tensor("ps%d" % i, [128, 512], F32)) for i in range(8)]
        self.rbank = [[Res("ps%d" % i)] for i in range(8)]
        self.banks_b = [b.bitcast(BF16) for b in self.banks]
        self.bank_i = 0
        self.held = set()

    def view(self, off_words, nwords, dtp=F32, shape=None):
        ap = self.arena[:, off_words:off_words + nwords]
        if dtp is not F32:
            ap = ap.bitcast(dtp)
        if shape is not None and len(shape) == 2:
            ap = ap.rearrange("p (a b) -> p a b", a=shape[0], b=shape[1])
        elif shape is not None and len(shape) == 3:
            ap = ap.rearrange("p (a b c) -> p a b c", a=shape[0], b=shape[1], c=shape[2])
        return ap

    def _next_bank(self, hold):
        while self.bank_i in self.held:
            self.bank_i = (self.bank_i + 1) % 8
        i = self.bank_i
        self.bank_i = (self.bank_i + 1) % 8
        if hold:
            self.held.add(i)
        return i

    def bank(self, hold=False):
        i = self._next_bank(hold)
        return self.banks[i], self.rbank[i]

    def bankb(self, hold=False):
        i = self._next_bank(hold)
        return self.banks_b[i], self.rbank[i]

    def unhold(self, rps):
        self.held.discard(int(rps[0].name[2:]))

    def weight_lists(self):
        la, lb = [], []
        for s in range(self.nseq):
            for l in range(self.nl):
                for rep in range(2):
                    for ci in range(8):
                        la.append(self.d_win[l, ci])
                for ci in range(8, NWIN):
                    la.append(self.d_win[l, ci])
                for oc in range(KC):
                    la.append(self.d_wout[l, oc])
                for half in range(2):
                    for j in range(FC):
                        la.append(self.d_wg[l, j])
                        la.append(self.d_wu[l, j])
                    for oc in range(KC):
                        lb.append(self.d_wd[l, oc])
        self.listA, self.listB = la, lb
        self.issA = self.issB = 0
        self.conA = self.conB = 0
        self.doneA = self.doneB = 0

    def _pump(self, which):
        P = self.P
        if which == "A":
            while self.issA < len(self.listA) and self.issA - self.RA < self.doneA:
                m = self.issA
                slot = self.ringA[m % self.RA]
                src = self.listA[m]
                P.op("pool", lambda e, slot=slot, src=src: e.dma_start(
                    out=slot[:].rearrange("p k n -> p (k n)"), in_=src),
                    writes=[self.rA[m % self.RA]], dma_key=("A", m % self.RA))
                self.issA += 1
        else:
            while self.issB < len(self.listB) and self.issB - self.RB < self.doneB:
                m = self.issB
                slot = self.ringB[m % self.RB]
                src = self.listB[m]
                P.op("pool", lambda e, slot=slot, src=src: e.dma_start(
                    out=slot[:].rearrange("p k n -> p (k n)"), in_=src),
                    writes=[self.rB[m % self.RB]], dma_key=("B", m % self.RB))
                self.issB += 1

    def acqA(self):
        self._pump("A")
        n = self.conA
        assert n < self.issA, "ring A deadlock"
        self.conA += 1
        return self.ringA[n % self.RA], self.rA[n % self.RA]

    def relA(self, k=1):
        self.doneA += k
        self._pump("A")

    def acqB(self):
        self._pump("B")
        n = self.conB
        assert n < self.issB, "ring B deadlock"
        self.conB += 1
        return self.ringB[n % self.RB], self.rB[n % self.RB]

    def relB(self):
        self.doneB += 1
        self._pump("B")

    def tap(self, name, ap, shape, res, bf=False):
        if name not in self.taps:
            return
        d = self.nc.dram_tensor("tap_" + name, list(shape), F32, kind="ExternalOutput").ap()
        self.tap_out[name] = d
        q = "pool" if bf else "sp"
        self.P.op(q, lambda e: e.dma_start(out=d, in_=ap), reads=list(res), dma_key=("tap_" + q + ("_" + name if bf else "")))

    def mm8(self, ps, rps, slot, rslot, rhs_fn, nk=KC, col=slice(None)):
        P = self.P
        for k in range(nk):
            rhs, rr = rhs_fn(k)
            P.op("pe", lambda e, k=k, rhs=rhs: e.matmul(ps[:], slot[:, k, col], rhs, start=(k == 0), stop=(k == nk - 1)),
                 reads=[rslot, rr], writes=rps)

    def stats_rstd(self, srcs, ones_idx, eps, rstd, rrstd, sq, rsq):
        P = self.P
        ps, rps = self.bank()
        W = 512
        n = len(srcs)
        for c, (src, rs) in enumerate(srcs):
            sqa, sqr = sq[c % len(sq)]
            P.op("act", lambda e, src=src, sqa=sqa: e.activation(out=sqa, in_=src, func=AF.Square),
                 reads=[rs], writes=[sqr])
            P.op("pe", lambda e, c=c, sqa=sqa: e.matmul(ps[:, 0:W], self.ones_b[:, ones_idx, :], sqa,
                                                       start=(c == 0), stop=(c == n - 1)),
                 reads=[sqr, self.rC], writes=rps)
        P.op("act", lambda e: e.activation(out=rstd, in_=ps[:, 0:W], func=AF.Sqrt, bias=float(eps), scale=1.0),
             reads=rps, writes=[rrstd])
        P.op("dve", lambda e: e.reciprocal(rstd, rstd), reads=[rrstd], writes=[rrstd])

    def setup(self):
        P = self.P
        nc = self.nc
        P.op("sp", lambda e: e.dma_start(out=self.cmat[:], in_=self.d_cmat), writes=[self.rC], dma_key="c")
        P.op("sp", lambda e: e.dma_start(out=self.pp[:], in_=self.d_pp), writes=[self.rC], dma_key="c")
        P.op("sp", lambda e: e.dma_start(out=self.gdnc[:], in_=self.d_gdnc.partition_broadcast(128)),
             writes=[self.rC], dma_key="c")
        P.op("dve", lambda e: e.tensor_copy(self.ident_b[:], self.ident_f), reads=[self.rC], writes=[self.rC])
        P.op("dve", lambda e: e.memset(self.ones_b[:, 0, :], 1.0), writes=[self.rC])
        P.op("dve", lambda e: e.memset(self.ones_b[:, 1, :], 1.0 / 1024), writes=[self.rC])
        P.op("dve", lambda e: e.memset(self.ones_b[:, 2, :], 1.0 / 256), writes=[self.rC])
        P.op("dve", lambda e: e.memset(self.ones_b[:, 3, :], 1.0 / 128), writes=[self.rC])
        P.op("dve", lambda e: e.memset(self.ones_f[:], 1.0), writes=[self.rC])

    def ppcol(self, l, off, n=1):
        b = l * PP_PER_L + off
        return self.pp[:, b:b + n]


    def load_x(self, s):
        P = self.P
        for c in range(KC):
            P.op("sp", lambda e, c=c: e.dma_start(out=self.xT[:, c, :], in_=self.d_x[s, c * 128:(c + 1) * 128, :]),
                 writes=self.rX[c], dma_key=("x", c))

    def store_x(self, s):
        P = self.P
        for c in range(KC):
            P.op("sp", lambda e, c=c: e.dma_start(out=self.d_out[s, c * 128:(c + 1) * 128, :], in_=self.xT[:, c, :]),
                 reads=self.rX[c], dma_key=("o", c))

    def prenorm(self, l, gain_off, hT, rH, tts, sq, rstd_bufs):
        P = self.P
        for li, tt in enumerate(tts):
            tok = slice(tt * 512, (tt + 1) * 512)
            rstd, rr = rstd_bufs[li % len(rstd_bufs)]
            self.stats_rstd([(self.xT[:, c, tok], self.rX[c][tt]) for c in range(KC)], 1, RMS_EPS, rstd, rr, sq, None)
            for c in range(KC):
                P.op("dve", lambda e, c=c, tok=tok, li=li, rstd=rstd: e.scalar_tensor_tensor(
                    out=hT[:, c, li * 512:(li + 1) * 512], in0=self.xT[:, c, tok], scalar=self.ppcol(l, gain_off + c),
                    in1=rstd, op0=ALU.mult, op1=ALU.mult),
                    reads=[self.rX[c][tt], rr, self.rC], writes=[rH[c][li]])

    def postnorm_add(self, l, gain_off, f_fn, tts, sq, rstd_bufs, tmp_bufs):
        P = self.P
        for li, tt in enumerate(tts):
            tok = slice(tt * 512, (tt + 1) * 512)
            rstd, rr = rstd_bufs[li % len(rstd_bufs)]
            self.stats_rstd([f_fn(c, li) for c in range(KC)], 1, RMS_EPS, rstd, rr, sq, None)
            for c in range(KC):
                fa, fr = f_fn(c, li)
                tmp, rt = tmp_bufs[c % len(tmp_bufs)]
                P.op("dve", lambda e, c=c, fa=fa, tmp=tmp, rstd=rstd: e.scalar_tensor_tensor(
                    out=tmp, in0=fa, scalar=self.ppcol(l, gain_off + c), in1=rstd, op0=ALU.mult, op1=ALU.mult),
                    reads=[fr, rr, self.rC], writes=[rt])
                P.op("dve", lambda e, c=c, tok=tok, tmp=tmp: e.tensor_tensor(
                    out=self.xT[:, c, tok], in0=self.xT[:, c, tok], in1=tmp, op=ALU.add),
                    reads=[rt, self.rX[c][tt]], writes=[self.rX[c][tt]])

    def mixer(self, l):
        P = self.P
        HOFF, MOFF, SOFF = 0, 8192, 16384
        hT = self.view(HOFF, 8192, BF16, (KC, S))
        mixT = self.view(MOFF, 8192, BF16, (KC, S))
        rH = [[Res("h%d_%d" % (c, t)) for t in range(4)] for c in range(KC)]
        rM = [[Res("m%d_%d" % (c, t)) for t in range(4)] for c in range(KC)]
        self.hT, self.rH, self.mixT, self.rM = hT, rH, mixT, rM

        def hrhs(tt):
            return lambda k: (hT[:, k, tt * 512:(tt + 1) * 512], rH[k][tt])

        so = SOFF
        sq = [(self.view(so + i * 256, 256, BF16), Res("sq%d" % i)) for i in range(2)]
        rstdb = [(self.view(so + 512 + i * 512, 512), Res("rstd%d" % i)) for i in range(2)]
        P.barrier()
        self.prenorm(l, 0, hT, rH, [0, 1, 2, 3], sq, rstdb)
        if self.stop == "pre":
            return
        self.tap("hT", hT.rearrange("p c t -> p (c t)"), [128, KC * S], [r for rr in rH for r in rr], bf=True)

        P.barrier()
        csb = self.view(SOFF, 2048, F32, (2, 1024))
        rcs = Res("cs")
        t1 = [(self.view(SOFF + 2048 + i * 512, 512), Res("t1_%d" % i)) for i in range(2)]
        QK = self.view(SOFF + 3072, 4096, BF16, (4, S))
        rQK = [[Res("qk%d_%d" % (i, t)) for t in range(4)] for i in range(4)]
        for half in range(2):
            P.op("sp", lambda e, half=half: e.dma_start(out=csb[:, 0, :], in_=self.d_cs[0, :, half * 1024:(half + 1) * 1024]),
                 writes=[rcs], dma_key="cs")
            P.op("sp", lambda e, half=half: e.dma_start(out=csb[:, 1, :], in_=self.d_cs[1, :, half * 1024:(half + 1) * 1024]),
                 writes=[rcs], dma_key="cs")
            for i in range(4):
                sa, ra = self.acqA()
                sbw, rb = self.acqA()
                for t2 in range(2):
                    tt = half * 2 + t2
                    pa, rpa = self.bank()
                    pb, rpb = self.bank()
                    self.mm8(pa, rpa, sa, ra, hrhs(tt))
                    self.mm8(pb, rpb, sbw, rb, hrhs(tt))
                    (ta, rta), (tb, rtb) = t1
                    lc = slice(t2 * 512, (t2 + 1) * 512)
                    P.op("dve", lambda e, pa=pa, ta=ta, lc=lc: e.tensor_tensor(out=ta, in0=pa[:], in1=csb[:, 0, lc], op=ALU.mult),
                         reads=rpa + [rcs], writes=[rta])
                    P.op("dve", lambda e, pb=pb, tb=tb, lc=lc: e.tensor_tensor(out=tb, in0=pb[:], in1=csb[:, 1, lc], op=ALU.mult),
                         reads=rpb + [rcs], writes=[rtb])
                    P.op("dve", lambda e, i=i, tt=tt, ta=ta, tb=tb: e.tensor_tensor(
                        out=QK[:, i, tt * 512:(tt + 1) * 512], in0=ta, in1=tb, op=ALU.add),
                        reads=[rta, rtb], writes=[rQK[i][tt]])
                self.relA(2)
        if self.stop == "rot":
            return
        self.tap("QK", QK.rearrange("p c t -> p (c t)"), [128, 4 * S], [r for rr in rQK for r in rr], bf=True)

        P.barrier()
        VW = 16 * 4 * 66
        Vp = self.view(SOFF, VW // 2, BF16, (16, 4, 66))
        rV = [Res("v%d" % t) for t in range(16)]
        rV1 = Res("vones")
        P.op("dve", lambda e: e.memset(Vp[:, :, :, 64:65], 1.0), writes=[rV1])
        s0, r0 = self.acqA()
        s1, r1 = self.acqA()
        for t16 in range(16):
            pv, rpv = self.bank()
            for vc, (sv, rv) in enumerate(((s0, r0), (s1, r1))):
                for k in range(KC):
                    P.op("pe", lambda e, k=k, vc=vc, sv=sv, pv=pv, t16=t16: e.matmul(
                        pv[:, vc * 128:(vc + 1) * 128], hT[:, k, t16 * 128:(t16 + 1) * 128], sv[:, k, :],
                        start=(k == 0), stop=(k == KC - 1)),
                        reads=[rv, rH[k][t16 // 4]], writes=rpv)
            P.op("act", lambda e, pv=pv, t16=t16: e.activation(
                out=Vp[:, t16, :, 0:64], in_=pv[:, 0:256].rearrange("p (h d) -> p h d", h=4, d=64), func=AF.Copy),
                reads=rpv, writes=[rV[t16]])
        self.relA(2)

        if self.stop == "v":
            return
        msb = self.view(SOFF + 8704, MS_W // 2, BF16)
        rms = Res("ms")
        PB = [(self.view(SOFF + 7168 + i * 256, 256, BF16), Res("pb%d" % i)) for i in range(3)]
        atm = self.view(SOFF + 7168 + 768, 512, BF16, (4, 256))
        ratm = Res("atm")
        rden = self.view(SOFF + 7168 + 1280, 4)
        rrden = Res("rden")
        stg = self.view(MOFF, MS_W)
        rstg = Res("stg")
        P.op("sp", lambda e: e.dma_start(out=stg, in_=self.d_ms), writes=[rstg], dma_key="cs")
        P.op("act", lambda e: e.activation(out=msb, in_=stg, func=AF.Copy), reads=[rstg], writes=[rms])
        P.barrier()
        for Q in range(4):
            qs_tok = slice(Q * 512, (Q + 1) * 512)
            for h in range(4):
                cq = h // 2
                base = 64 * (h % 2)
                po, rpo = self.bank(hold=True)
                pov = po[:, 0:264].rearrange("p (a b) -> p a b", a=4, b=66)
                nkt = 4 * Q + 4
                def smm(kt, cq=cq, base=base, qs_tok=qs_tok, Q=Q):
                    pS, rpS = self.bank()
                    P.op("pe", lambda e, pS=pS, cq=cq, base=base, kt=kt, qs_tok=qs_tok: e.matmul(
                        pS[:], QK[base:base + 64, 2 + cq, kt * 128:(kt + 1) * 128], QK[base:base + 64, cq, qs_tok],
                        start=True, stop=True),
                        reads=[rQK[2 + cq][kt // 4], rQK[cq][Q]], writes=rpS)
                    return pS, rpS

                nxtS = smm(0)
                for kt in range(nkt):
                    pS, rpS = nxtS
                    if kt + 1 < nkt:
                        nxtS = smm(kt + 1)
                    pb, rpb = PB[(kt) % 3]
                    P.op("act", lambda e, pS=pS, pb=pb: e.activation(out=pb, in_=pS[:], func=AF.Exp, scale=0.125),
                         reads=rpS, writes=[rpb])
                    off = Q * 512 - kt * 128 + MS_OFF
                    P.op("dve", lambda e, pb=pb, off=off: e.tensor_tensor(out=pb, in0=pb, in1=msb[:, off:off + 512], op=ALU.mult),
                         reads=[rpb, rms], writes=[rpb])
                    for qs in range(4):
                        last = 4 * Q + qs
                        if kt > last:
                            continue
                        P.op("pe", lambda e, pov=pov, qs=qs, pb=pb, kt=kt, h=h, last=last: e.matmul(
                            pov[:, qs, 0:65], pb[:, qs * 128:(qs + 1) * 128], Vp[:, kt, h, 0:65],
                            start=(kt == 0 and qs == 0), stop=(kt == last), skip_group_check=True),
                            reads=[rpb, rV[kt], rV1], writes=rpo)
                P.op("dve", lambda e, pov=pov: e.reciprocal(rden.unsqueeze(2), pov[:, :, 64:65]),
                     reads=rpo, writes=[rrden])
                P.op("dve", lambda e, pov=pov, h=h: e.tensor_tensor(
                    out=atm[:, :, h * 64:(h + 1) * 64], in0=pov[:, :, 0:64],
                    in1=rden.unsqueeze(2).to_broadcast([128, 4, 64]), op=ALU.mult),
                    reads=rpo + [rrden], writes=[ratm])
                self.unhold(rpo)
            for c2 in range(2):
                pt, rpt = self.bankb()
                for qs in range(4):
                    P.op("pe", lambda e, qs=qs, c2=c2, pt=pt: e.transpose(
                        pt[:, qs * 128:(qs + 1) * 128], atm[:, qs, c2 * 128:(c2 + 1) * 128], self.ident_b[:]),
                        reads=[ratm, self.rC], writes=rpt)
                P.op("act", lambda e, c2=c2, qs_tok=qs_tok, pt=pt: e.activation(out=mixT[:, c2, qs_tok], in_=pt[:, 0:512], func=AF.Copy),
                     reads=rpt, writes=[rM[c2][Q]])
        if self.stop == "attn":
            return
        self.tap("mix_attn", mixT[:, 0:2, :].rearrange("p c t -> p (c t)"), [128, 2 * S], rM[0] + rM[1], bf=True)

        P.barrier()
        YW = 2080
        ybf = self.view(SOFF, YW, BF16, (2, YW))
        rY = [[Res("y%d_%d" % (c, t)) for t in range(4)] for c in range(2)]
        rYp = Res("ypad")
        dg = self.view(SOFF + 2080, 3968, BF16, (2, 31, 128))
        rdg = Res("dg")
        yc = self.view(SOFF + 6048, 4096, F32, (2, S))
        rYc = [[Res("yc%d_%d" % (c, t)) for t in range(4)] for c in range(2)]
        ctmp = [(self.view(SOFF + 10144 + i * 512, 512), Res("ctmp%d" % i)) for i in range(2)]
        csq = [(self.view(SOFF + 11168 + i * 256, 256, BF16), Res("csq%d" % i)) for i in range(4)]
        P.op("dve", lambda e: e.memset(ybf[:, :, 0:30], 0.0), writes=[rYp])
        for cc in range(2):
            P.op("dve", lambda e, cc=cc: e.tensor_tensor(
                out=dg[:, cc, :, :], in0=self.ident_b[:].unsqueeze(1).to_broadcast([128, 31, 128]),
                in1=self.ppcol(l, 32 + cc * 31, 31).unsqueeze(2).to_broadcast([128, 31, 128]), op=ALU.mult),
                reads=[self.rC], writes=[rdg])
        for cc in range(2):
            sv, rv = self.acqA()
            sg, rg = self.acqA()
            for tt in range(4):
                pv, rpv = self.bank()
                pg, rpg = self.bank()
                self.mm8(pv, rpv, sv, rv, hrhs(tt))
                self.mm8(pg, rpg, sg, rg, hrhs(tt))
                tmp, rt = ctmp[tt % 2]
                P.op("act", lambda e, pg=pg, tmp=tmp: e.activation(out=tmp, in_=pg[:], func=AF.Sigmoid), reads=rpg, writes=[rt])
                P.op("dve", lambda e, pv=pv, tmp=tmp, cc=cc, tt=tt: e.tensor_tensor(
                    out=ybf[:, cc, 30 + tt * 512:30 + (tt + 1) * 512], in0=pv[:], in1=tmp, op=ALU.mult),
                    reads=rpv + [rt], writes=[rY[cc][tt]])
            self.relA(2)
        for cc in range(2):
            for tt in range(4):
                pc, rpc = self.bank()
                rd = [rY[cc][tt], rdg, rYp] + ([rY[cc][tt - 1]] if tt > 0 else [])
                for j in range(31):
                    P.op("pe", lambda e, pc=pc, cc=cc, tt=tt, j=j: e.matmul(
                        pc[:], dg[:, cc, j, :], ybf[:, cc, tt * 512 + j:tt * 512 + j + 512], start=(j == 0), stop=(j == 30)),
                        reads=rd, writes=rpc)
                P.op("act", lambda e, pc=pc, cc=cc, tt=tt: e.activation(
                    out=yc[:, cc, tt * 512:(tt + 1) * 512], in_=pc[:], func=AF.Identity, bias=self.ppcol(l, 94 + cc), scale=1.0),
                    reads=rpc + [self.rC], writes=[rYc[cc][tt]])
        for tt in range(4):
            tok = slice(tt * 512, (tt + 1) * 512)
            pm, rpm = self.bank()
            pq, rpq = self.bank()
            for cc in range(2):
                yb, ryb = csq[cc]
                ys, rys = csq[2 + cc]
                P.op("dve", lambda e, yb=yb, cc=cc, tok=tok: e.tensor_copy(yb, yc[:, cc, tok]), reads=[rYc[cc][tt]], writes=[ryb])
                P.op("act", lambda e, ys=ys, cc=cc, tok=tok: e.activation(out=ys, in_=yc[:, cc, tok], func=AF.Square),
                     reads=[rYc[cc][tt]], writes=[rys])
                P.op("pe", lambda e, pm=pm, yb=yb, cc=cc: e.matmul(pm[:], self.ones_b[:, 2, :], yb, start=(cc == 0), stop=(cc == 1)),
                     reads=[ryb, self.rC], writes=rpm)
                P.op("pe", lambda e, pq=pq, ys=ys, cc=cc: e.matmul(pq[:], self.ones_b[:, 2, :], ys, start=(cc == 0), stop=(cc == 1)),
                     reads=[rys, self.rC], writes=rpq)
            (ta, rta), (tb, rtb) = ctmp
            P.op("act", lambda e, pm=pm, ta=ta: e.activation(out=ta, in_=pm[:], func=AF.Square), reads=rpm, writes=[rta])
            P.op("dve", lambda e, pq=pq, ta=ta: e.tensor_tensor(out=ta, in0=pq[:], in1=ta, op=ALU.subtract), reads=rpq + [rta], writes=[rta])
            P.op("act", lambda e, ta=ta: e.activation(out=ta, in_=ta, func=AF.Sqrt, bias=float(LN_EPS), scale=1.0), reads=[rta], writes=[rta])
            P.op("dve", lambda e, ta=ta: e.reciprocal(ta, ta), reads=[rta], writes=[rta])
            for cc in range(2):
                P.op("dve", lambda e, pm=pm, tb=tb, cc=cc, tok=tok: e.tensor_tensor(out=tb, in0=yc[:, cc, tok], in1=pm[:], op=ALU.subtract),
                     reads=rpm + [rYc[cc][tt]], writes=[rtb])
                P.op("dve", lambda e, ta=ta, tb=tb: e.tensor_tensor(out=tb, in0=tb, in1=ta, op=ALU.mult), reads=[rta, rtb], writes=[rtb])
                P.op("act", lambda e, tb=tb, cc=cc, tok=tok: e.activation(
                    out=mixT[:, 2 + cc, tok], in_=tb, func=AF.Silu, bias=self.ppcol(l, 98 + cc), scale=self.ppcol(l, 96 + cc)),
                    reads=[rtb, self.rC], writes=[rM[2 + cc][tt]])
        if self.stop == "conv":
            return
        self.tap("mix_conv", mixT[:, 2:4, :].rearrange("p c t -> p (c t)"), [128, 2 * S], rM[2] + rM[3], bf=True)

        P.barrier()
        self.gdn(l, SOFF)
        if self.stop == "gdn":
            return
        self.tap("mix_gdn", mixT[:, 4:8, :].rearrange("p c t -> p (c t)"), [128, 4 * S], rM[4] + rM[5] + rM[6] + rM[7], bf=True)

        P.barrier()
        fA = self.view(HOFF, 8192, F32, (4, S))
        fB = self.view(SOFF, 8192, F32, (4, S))
        rF = [[Res("f%d_%d" % (c, t)) for t in range(4)] for c in range(KC)]

        def fap(c, tt):
            t = fA if c < 4 else fB
            return t[:, c % 4, tt * 512:(tt + 1) * 512], rF[c][tt]

        so = SOFF + 8192
        sq = [(self.view(so + i * 256, 256, BF16), Res("wsq%d" % i)) for i in range(2)]
        rstdb = [(self.view(so + 512 + i * 512, 512), Res("wrstd%d" % i)) for i in range(2)]
        tmpb = [(self.view(so + 1536 + i * 512, 512), Res("wtmp%d" % i)) for i in range(2)]
        for oc in range(KC):
            sw, rw = self.acqA()
            for tt in range(4):
                pf, rpf = self.bank()
                self.mm8(pf, rpf, sw, rw, lambda k, tt=tt: (mixT[:, k, tt * 512:(tt + 1) * 512], rM[k][tt]))
                fa, fr = fap(oc, tt)
                if (oc + tt) % 2 == 0:
                    P.op("act", lambda e, pf=pf, fa=fa: e.activation(out=fa, in_=pf[:], func=AF.Copy), reads=rpf, writes=[fr])
                else:
                    P.op("dve", lambda e, pf=pf, fa=fa: e.tensor_copy(fa, pf[:]), reads=rpf, writes=[fr])
            self.relA()
        self.postnorm_add(l, 8, fap, [0, 1, 2, 3], sq, rstdb, tmpb)
        self.tap("x_mid", self.xT[:].rearrange("p c t -> p (c t)"), [128, KC * S], [r for rr in self.rX for r in rr])

    def gdn(self, l, SOFF):
        P = self.P
        hT, rH, mixT, rM = self.hT, self.rH, self.mixT, self.rM
        o = SOFF
        zbuf = self.view(o, 2052)
        o += 2052
        acc = self.view(o, 2048)
        o += 2048
        qn = self.view(o, 1024, BF16)
        o += 1024
        kn = self.view(o, 1024, BF16)
        o += 1024
        vT = self.view(o, 1024, BF16)
        o += 1024
        sgT = self.view(o, 1024, BF16)
        o += 1024
        small = o
        rz = [Res("z%d" % t) for t in range(4)]
        rzp = Res("zpad")
        racc = [Res("acc%d" % t) for t in range(4)]
        rqn = [Res("qn%d" % t) for t in range(4)]
        rkn = [Res("kn%d" % t) for t in range(4)]
        rvT = [Res("vT%d" % t) for t in range(4)]
        rsg = [Res("sg%d" % t) for t in range(4)]

        cnt = [small]

        def sm(nwords, dtp=F32, shape=None, name="t"):
            ap = self.view(cnt[0], nwords, dtp, shape)
            cnt[0] += nwords
            return ap, Res(name)

        ab, rab = sm(128, F32, None, "ab")
        abv = ab.rearrange("p (c e) -> p c e", c=16, e=8)
        xg, rxg = sm(64, name="xg")
        t64a, rt64a = sm(64, name="t64a")
        t64b, rt64b = sm(64, name="t64b")
        g_, rg_ = sm(64, name="g")
        beta, rbeta = sm(64, name="beta")
        nbeta, rnbeta = sm(64, name="nbeta")
        gc, rgc = sm(64, name="gc")
        ngc, rngc = sm(64, name="ngc")
        bg, rbg = sm(64, name="bg")
        kds, rkds = sm(64, name="kds")
        gle, rgle = sm(64, name="gle")
        sqb = [sm(256, BF16, None, "gsq%d" % i) for i in range(1)]
        rinv = [sm(512, F32, None, "rinv%d" % i) for i in range(1)]
        dgc, rdgc = sm(128, name="dgc")
        tl, rtl = sm(128, name="tl")
        tu, rtu = tl, rtl
        El, rEl = sm(128, name="El")
        Eu, rEu = sm(128, name="Eu")
        gamr, rgamr = sm(128, name="gamr")
        knf, rknf = sm(128, name="knf")
        kd, rkd = sm(64, BF16, None, "kd")
        vtm, rvtm = sm(64, BF16, None, "vtm")
        rr_, rrr = sm(128, name="r")
        gam, rgam = sm(64, name="gam")
        Ab = [sm(128, F32, None, "A%d" % i) for i in range(2)]
        Bb = [sm(128, F32, None, "B%d" % i) for i in range(2)]
        TTb = [sm(128, F32, None, "TT%d" % i) for i in range(2)]
        intraT, rintra = sm(64, BF16, None, "intraT")
        qg, rqg = sm(64, BF16, None, "qg")
        vnew, rvnew = sm(64, BF16, None, "vnew")
        Sf, rSf = sm(128, name="Sf")
        Sb, rSb = sm(64, BF16, None, "Sb")
        ss, rss = sm(64, name="ss")
        onb, ronb = sm(64, BF16, None, "on")
        junk, rjunk = tl, rtl
        assert cnt[0] <= SOFF + 12288, cnt[0]

        def hrhs(tt):
            return lambda k: (hT[:, k, tt * 512:(tt + 1) * 512], rH[k][tt])

        gview = self.gdnc[:, l * 128:(l + 1) * 128]
        alog_t = gview[:, 0:64]
        dtb_t = gview[:, 64:128]

        sab, rsab = self.acqA()
        pab, rpab = self.bank()
        for c16 in range(16):
            for k in range(KC):
                P.op("pe", lambda e, k=k, c16=c16: e.matmul(
                    pab[:, c16 * 8:(c16 + 1) * 8], hT[:, k, c16 * 128:(c16 + 1) * 128], sab[:, k, 0:8],
                    start=(k == 0), stop=(k == KC - 1)),
                    reads=[rsab, rH[k][c16 // 4]], writes=rpab)
        self.relA()
        P.op("act", lambda e: e.activation(out=ab, in_=pab[:, 0:128], func=AF.Copy), reads=rpab, writes=[rab])
        v3 = lambda t: t.rearrange("p (c h) -> p c h", c=16, h=4)
        P.op("dve", lambda e: e.tensor_tensor(out=v3(xg), in0=abv[:, :, 0:4], in1=v3(dtb_t), op=ALU.add), reads=[rab, self.rC], writes=[rxg])
        P.op("act", lambda e: e.activation(out=t64a, in_=xg, func=AF.Abs), reads=[rxg], writes=[rt64a])
        P.op("act", lambda e: e.activation(out=t64a, in_=t64a, func=AF.Exp, scale=-1.0), reads=[rt64a], writes=[rt64a])
        P.op("act", lambda e: e.activation(out=t64a, in_=t64a, func=AF.Ln, bias=1.0, scale=1.0), reads=[rt64a], writes=[rt64a])
        P.op("dve", lambda e: e.tensor_scalar_max(out=t64b, in0=xg, scalar1=0.0), reads=[rxg], writes=[rt64b])
        P.op("dve", lambda e: e.tensor_tensor(out=t64b, in0=t64b, in1=t64a, op=ALU.add), reads=[rt64a, rt64b], writes=[rt64b])
        P.op("act", lambda e: e.activation(out=t64a, in_=alog_t, func=AF.Exp), reads=[self.rC, rt64a], writes=[rt64a])
        P.op("dve", lambda e: e.scalar_tensor_tensor(out=g_, in0=t64b, scalar=-1.0, in1=t64a, op0=ALU.mult, op1=ALU.mult),
             reads=[rt64a, rt64b], writes=[rg_])
        P.op("act", lambda e: e.activation(out=v3(beta), in_=abv[:, :, 4:8], func=AF.Sigmoid), reads=[rab], writes=[rbeta])
        P.op("dve", lambda e: e.tensor_scalar_mul(out=nbeta, in0=beta, scalar1=-1.0), reads=[rbeta], writes=[rnbeta])
        pg, rpg0 = self.bank()
        pg2, rpg1 = self.bank()
        P.op("pe", lambda e: e.matmul(pg[:, 0:64], self.tri_f, g_, start=True, stop=True), reads=[rg_, self.rC], writes=rpg0)
        P.op("pe", lambda e: e.matmul(pg2[:, 128:192], self.ones_f[:], g_, start=True, stop=True), reads=[rg_, self.rC], writes=rpg1)
        P.op("act", lambda e: e.activation(out=gc, in_=pg[:, 0:64], func=AF.Copy), reads=rpg0, writes=[rgc])
        P.op("dve", lambda e: e.tensor_scalar_mul(out=ngc, in0=pg[:, 0:64], scalar1=-1.0), reads=rpg0, writes=[rngc])
        P.op("act", lambda e: e.activation(out=gam, in_=pg[:, 0:64], func=AF.Exp), reads=rpg0, writes=[rgam])
        P.op("act", lambda e: e.activation(out=bg, in_=pg[:, 0:64], func=AF.Exp), reads=rpg0, writes=[rbg])
        P.op("dve", lambda e: e.tensor_tensor(out=bg, in0=bg, in1=beta, op=ALU.mult), reads=[rbg, rbeta], writes=[rbg])
        P.op("dve", lambda e: e.tensor_tensor(out=kds, in0=pg2[:, 128:192], in1=gc, op=ALU.subtract), reads=rpg1 + [rgc], writes=[rkds])
        P.op("act", lambda e: e.activation(out=kds, in_=kds, func=AF.Exp), reads=[rkds], writes=[rkds])
        P.op("act", lambda e: e.activation(out=gle, in_=pg2[:, 128:192], func=AF.Exp), reads=rpg1, writes=[rgle])
        P.op("dve", lambda e: e.memset(zbuf[:, 0:3], 0.0), writes=[rzp])
        if "gates" in self.taps:
            self.tap("g", g_, [128, 64], [rg_])
            self.tap("gc", gc, [128, 64], [rgc])
            self.tap("beta", beta, [128, 64], [rbeta])

        for h in range(4):
            for xi in range(3):
                sw, rw = self.acqA()
                for tt in range(4):
                    pz, rpz = self.bank()
                    self.mm8(pz, rpz, sw, rw, hrhs(tt))
                    P.op("act", lambda e, pz=pz, tt=tt: e.activation(out=zbuf[:, 3 + tt * 512:3 + (tt + 1) * 512], in_=pz[:], func=AF.Copy),
                         reads=rpz, writes=[rz[tt]])
                self.relA()
                wc = 100 + (xi * 4 + h) * 4
                for tt in range(4):
                    rd = [rz[tt], rzp, self.rC] + ([rz[tt - 1]] if tt > 0 else [])
                    a_ = acc[:, tt * 512:(tt + 1) * 512]
                    P.op("dve", lambda e, a_=a_, tt=tt, wc=wc: e.tensor_scalar_mul(out=a_, in0=zbuf[:, tt * 512 + 3:tt * 512 + 515], scalar1=self.ppcol(l, wc + 3)),
                         reads=rd, writes=[racc[tt]])
                    for j in (2, 1, 0):
                        P.op("dve", lambda e, a_=a_, tt=tt, wc=wc, j=j: e.scalar_tensor_tensor(
                            out=a_, in0=zbuf[:, tt * 512 + j:tt * 512 + j + 512], scalar=self.ppcol(l, wc + j), in1=a_, op0=ALU.mult, op1=ALU.add),
                            reads=rd + [racc[tt]], writes=[racc[tt]])
                    if xi == 2:
                        P.op("act", lambda e, a_=a_, tt=tt: e.activation(out=vT[:, tt * 512:(tt + 1) * 512], in_=a_, func=AF.Silu),
                             reads=[racc[tt]], writes=[rvT[tt]])
                        continue
                    P.op("act", lambda e, a_=a_: e.activation(out=a_, in_=a_, func=AF.Silu), reads=[racc[tt]], writes=[racc[tt]])
                    sq_, rsq_ = sqb[0]
                    P.op("act", lambda e, a_=a_, sq_=sq_: e.activation(out=sq_, in_=a_, func=AF.Square), reads=[racc[tt]], writes=[rsq_])
                    pss, rpss = self.bank()
                    P.op("pe", lambda e, pss=pss, sq_=sq_: e.matmul(pss[:], self.ones_b[:, 0, :], sq_, start=True, stop=True),
                         reads=[rsq_, self.rC], writes=rpss)
                    ri, rri = rinv[0]
                    P.op("act", lambda e, pss=pss, ri=ri: e.activation(out=ri, in_=pss[:], func=AF.Sqrt, bias=float(L2_EPS), scale=1.0),
                         reads=rpss, writes=[rri])
                    P.op("dve", lambda e, ri=ri: e.reciprocal(ri, ri), reads=[rri], writes=[rri])
                    dst, rdst = (qn, rqn) if xi == 0 else (kn, rkn)
                    scl = (128.0 ** -0.5) if xi == 0 else 1.0
                    P.op("dve", lambda e, a_=a_, ri=ri, dst=dst, tt=tt, scl=scl: e.scalar_tensor_tensor(
                        out=dst[:, tt * 512:(tt + 1) * 512], in0=a_, scalar=scl, in1=ri, op0=ALU.mult, op1=ALU.mult),
                        reads=[racc[tt], rri], writes=[rdst[tt]])
            sw, rw = self.acqA()
            for tt in range(4):
                pz, rpz = self.bank()
                self.mm8(pz, rpz, sw, rw, hrhs(tt))
                P.op("act", lambda e, pz=pz, tt=tt: e.activation(out=sgT[:, tt * 512:(tt + 1) * 512], in_=pz[:], func=AF.Silu),
                     reads=rpz, writes=[rsg[tt]])
            self.relA()
            if "gdn_qkv" in self.taps and h == 0:
                self.tap("qn0", qn, [128, S], rqn, bf=True)
                self.tap("kn0", kn, [128, S], rkn, bf=True)
                self.tap("vT0", vT, [128, S], rvT, bf=True)

            for c in range(16):
                tok = slice(c * 128, (c + 1) * 128)
                t4 = c // 4
                col = c * 4 + h
                cs1 = slice(col, col + 1)
                ptk, rptk = self.bankb()
                ptv, rptv = self.bankb()
                P.op("pe", lambda e, tok=tok, ptk=ptk: e.transpose(ptk[:, 0:128], kn[:, tok], self.ident_b[:]), reads=[rkn[t4], self.rC], writes=rptk)
                P.op("pe", lambda e, tok=tok, ptv=ptv: e.transpose(ptv[:, 0:128], vT[:, tok], self.ident_b[:]), reads=[rvT[t4], self.rC], writes=rptv)
                P.op("dve", lambda e, cs1=cs1, ptk=ptk: e.tensor_scalar_mul(out=kd, in0=ptk[:, 0:128], scalar1=kds[:, cs1]), reads=rptk + [rkds], writes=[rkd])
                P.op("act", lambda e, ptv=ptv: e.activation(out=vtm, in_=ptv[:, 0:128], func=AF.Copy), reads=rptv, writes=[rvtm])
                P.op("act", lambda e, tok=tok: e.activation(out=knf, in_=kn[:, tok], func=AF.Copy), reads=[rkn[t4]], writes=[rknf])
                pKK, rKK = self.bank()
                pQK, rQKp = self.bank()
                pGC, rGC = self.bank()
                P.op("pe", lambda e, pKK=pKK, tok=tok: e.matmul(pKK[:, 0:128], kn[:, tok], kn[:, tok], start=True, stop=True), reads=[rkn[t4]], writes=rKK)
                P.op("pe", lambda e, pQK=pQK, tok=tok: e.matmul(pQK[:, 0:128], kn[:, tok], qn[:, tok], start=True, stop=True), reads=[rkn[t4], rqn[t4]], writes=rQKp)
                P.op("dve", lambda e, cs1=cs1: e.tensor_scalar_mul(out=dgc, in0=self.ident_f, scalar1=gc[:, cs1]), reads=[rgc, self.rC], writes=[rdgc])
                P.op("pe", lambda e, pGC=pGC: e.matmul(pGC[:, 0:128], self.ones_f[:], dgc, start=True, stop=True), reads=[rdgc, self.rC], writes=rGC)
                gcr = pGC[:, 0:128]
                P.op("dve", lambda e, gcr=gcr: e.scalar_tensor_tensor(out=tl, in0=gcr, scalar=-1.0, in1=self.nmls, op0=ALU.mult, op1=ALU.add),
                     reads=rGC + [self.rC], writes=[rtl])
                P.op("act", lambda e, cs1=cs1: e.activation(out=El, in_=tl, func=AF.Exp, bias=gc[:, cs1], scale=1.0), reads=[rtl, rgc], writes=[rEl])
                P.op("dve", lambda e, gcr=gcr: e.tensor_tensor(out=tu, in0=gcr, in1=self.nmui, op=ALU.add), reads=rGC + [self.rC], writes=[rtu])
                P.op("act", lambda e, cs1=cs1: e.activation(out=Eu, in_=tu, func=AF.Exp, bias=ngc[:, cs1], scale=1.0), reads=[rtu, rngc], writes=[rEu])
                P.op("act", lambda e, gcr=gcr: e.activation(out=gamr, in_=gcr, func=AF.Exp), reads=rGC, writes=[rgamr])
                A0, rA0 = Ab[0]
                B0, rB0 = Bb[0]
                P.op("dve", lambda e, pKK=pKK, cs1=cs1, A0=A0: e.scalar_tensor_tensor(
                    out=A0, in0=pKK[:, 0:128], scalar=nbeta[:, cs1], in1=El, op0=ALU.mult, op1=ALU.mult),
                    reads=rKK + [rnbeta, rEl], writes=[rA0])
                pta, rpta = self.bank()
                P.op("pe", lambda e, A0=A0, pta=pta: e.transpose(pta[:, 0:128], A0, self.ident_f), reads=[rA0, self.rC], writes=rpta)
                P.op("act", lambda e, B0=B0, pta=pta: e.activation(out=B0, in_=pta[:, 0:128], func=AF.Copy), reads=rpta, writes=[rB0])
                P.op("dve", lambda e, pQK=pQK: e.tensor_tensor(out=intraT, in0=pQK[:, 0:128], in1=Eu, op=ALU.mult), reads=rQKp + [rEu], writes=[rintra])
                P.op("dve", lambda e, tok=tok: e.tensor_tensor(out=qg, in0=qn[:, tok], in1=gamr, op=ALU.mult), reads=[rqn[t4], rgamr], writes=[rqg])
                TT, rTT = TTb[0]
                P.op("dve", lambda e, TT=TT, B0=B0: e.tensor_tensor(out=TT, in0=self.ident_f, in1=B0, op=ALU.add), reads=[rB0, self.rC], writes=[rTT])
                cur = 0
                for k in range(1, 7):
                    Ap, rAp = Ab[cur]
                    Bp, rBp = Bb[cur]
                    An, rAn = Ab[1 - cur]
                    Bn, rBn = Bb[1 - cur]
                    TTp, rTTp = TTb[cur]
                    TTn, rTTn = TTb[1 - cur]
                    pa2, rpa2 = self.bank()
                    P.op("pe", lambda e, pa2=pa2, Ap=Ap, Bp=Bp: e.matmul(pa2[:, 0:128], Bp, Ap, start=True, stop=True), reads=[rAp, rBp], writes=rpa2)
                    if k < 6:
                        pb2, rpb2 = self.bank()
                        P.op("pe", lambda e, pb2=pb2, Ap=Ap, Bp=Bp: e.matmul(pb2[:, 0:128], Ap, Bp, start=True, stop=True), reads=[rAp, rBp], writes=rpb2)
                    P.op("act", lambda e, pa2=pa2, An=An: e.activation(out=An, in_=pa2[:, 0:128], func=AF.Copy), reads=rpa2, writes=[rAn])
                    if k < 6:
                        P.op("dve", lambda e, pb2=pb2, Bn=Bn: e.tensor_copy(Bn, pb2[:, 0:128]), reads=rpb2, writes=[rBn])
                    pt2, rpt2 = self.bank()
                    P.op("pe", lambda e, pt2=pt2, An=An, TTp=TTp: e.matmul(pt2[:, 0:128], An, TTp, start=True, stop=True), reads=[rAn, rTTp], writes=rpt2)
                    P.op("dve", lambda e, pt2=pt2, TTp=TTp, TTn=TTn: e.tensor_tensor(out=TTn, in0=TTp, in1=pt2[:, 0:128], op=ALU.add),
                         reads=rpt2 + [rTTp], writes=[rTTn])
                    cur = 1 - cur
                TT, rTT = TTb[cur]
                if c > 0:
                    pks, rks = self.bank()
                    P.op("pe", lambda e, pks=pks: e.matmul(pks[:, 0:128], knf, Sf, start=True, stop=True), reads=[rknf, rSf], writes=rks)
                    P.op("dve", lambda e, pks=pks, cs1=cs1: e.scalar_tensor_tensor(
                        out=rr_, in0=pks[:, 0:128], scalar=gam[:, cs1], in1=vtm, op0=ALU.mult, op1=ALU.subtract),
                        reads=rks + [rgam, rvtm], writes=[rrr])
                    P.op("dve", lambda e, cs1=cs1: e.tensor_scalar_mul(out=rr_, in0=rr_, scalar1=nbeta[:, cs1]), reads=[rrr, rnbeta], writes=[rrr])
                else:
                    P.op("dve", lambda e, cs1=cs1: e.tensor_scalar_mul(out=rr_, in0=vtm, scalar1=beta[:, cs1]), reads=[rvtm, rbeta], writes=[rrr])
                pWv, rWv = self.bank()
                pWo, rWo = self.bank()
                pWs, rWs = self.bank()
                P.op("pe", lambda e, pWv=pWv, TT=TT: e.matmul(pWv[:, 0:128], TT, rr_, start=True, stop=True), reads=[rTT, rrr], writes=rWv)
                P.op("act", lambda e, pWv=pWv: e.activation(out=vnew, in_=pWv[:, 0:128], func=AF.Copy), reads=rWv, writes=[rvnew])
                if c > 0:
                    P.op("pe", lambda e, pWo=pWo: e.matmul(pWo[:, 0:128], qg, Sb, start=True, stop=False), reads=[rqg, rSb], writes=rWo)
                P.op("pe", lambda e, pWo=pWo, c=c: e.matmul(pWo[:, 0:128], intraT, vnew, start=(c == 0), stop=True), reads=[rintra, rvnew], writes=rWo)
                P.op("pe", lambda e, pWs=pWs: e.matmul(pWs[:, 0:128], kd, vnew, start=True, stop=True), reads=[rkd, rvnew], writes=rWs)
                if c == 0:
                    P.op("dve", lambda e, pWs=pWs: e.tensor_copy(Sf, pWs[:, 0:128]), reads=rWs, writes=[rSf])
                else:
                    P.op("dve", lambda e, pWs=pWs, cs1=cs1: e.scalar_tensor_tensor(
                        out=Sf, in0=Sf, scalar=gle[:, cs1], in1=pWs[:, 0:128], op0=ALU.mult, op1=ALU.add),
                        reads=rWs + [rSf, rgle], writes=[rSf])
                P.op("act", lambda e: e.activation(out=Sb, in_=Sf, func=AF.Copy), reads=[rSf], writes=[rSb])
                po_ = pWo[:, 0:128]
                P.op("dve", lambda e: e.memset(ss[:, 0:1], 0.0), writes=[rss])
                P.op("act", lambda e, po_=po_: e.activation(out=junk, in_=po_, func=AF.Square, accum_out=ss[:, 0:1]), reads=rWo + [rss], writes=[rjunk, rss])
                P.op("act", lambda e: e.activation(out=ss[:, 1:2], in_=ss[:, 0:1], func=AF.Sqrt, bias=float(RMS_EPS), scale=1.0 / 128), reads=[rss], writes=[rss])
                P.op("dve", lambda e: e.reciprocal(ss[:, 1:2], ss[:, 1:2]), reads=[rss], writes=[rss])
                P.op("dve", lambda e, po_=po_: e.tensor_scalar_mul(out=onb, in0=po_, scalar1=ss[:, 1:2]), reads=rWo + [rss], writes=[ronb])
                pto, rpto = self.bankb()
                P.op("pe", lambda e, pto=pto: e.transpose(pto[:, 0:128], onb, self.ident_b[:]), reads=[ronb, self.rC], writes=rpto)
                P.op("dve", lambda e, tok=tok, h=h, pto=pto: e.scalar_tensor_tensor(
                    out=mixT[:, 4 + h, tok], in0=pto[:, 0:128], scalar=self.ppcol(l, 148), in1=sgT[:, tok], op0=ALU.mult, op1=ALU.mult),
                    reads=rpto + [rsg[t4], self.rC], writes=[rM[4 + h][t4]])

    def ffn(self, l):
        P = self.P
        P.barrier()
        hb = self.view(0, 4096, BF16, (KC, 1024))
        act = self.view(4096, 11264, BF16, (FC, 1024))
        f2 = self.view(15360, 8192, F32, (KC, 1024))
        so = 23552
        sq = [(self.view(so + i * 256, 256, BF16), Res("fsq%d" % i)) for i in range(2)]
        rstdb = [(self.view(so + 512 + i * 512, 512), Res("frstd%d" % i)) for i in range(2)]
        tmpb = [(self.view(so + 1536 + i * 512, 512), Res("ftmp%d" % i)) for i in range(2)]
        sgl = [(self.view(so + 2560 + i * 512, 512), Res("sgl%d" % i)) for i in range(2)]
        assert so + 3584 <= self.ARW
        rHb = [[Res("hb%d_%d" % (c, t)) for t in range(2)] for c in range(KC)]
        rAct = [[Res("a%d_%d" % (j, t)) for t in range(2)] for j in range(FC)]
        rF2 = [[Res("g%d_%d" % (c, t)) for t in range(2)] for c in range(KC)]
        for half in range(2):
            tts = [half * 2, half * 2 + 1]
            self.prenorm(l, 16, hb, rHb, tts, sq, rstdb)
            for j in range(FC):
                sg, rg = self.acqA()
                su, ru = self.acqA()
                for t2 in range(2):
                    pg, rpg = self.bank()
                    pu, rpu = self.bank()
                    rhs = lambda k, t2=t2: (hb[:, k, t2 * 512:(t2 + 1) * 512], rHb[k][t2])
                    self.mm8(pg, rpg, sg, rg, rhs)
                    self.mm8(pu, rpu, su, ru, rhs)
                    s_, rs_ = sgl[t2]
                    P.op("act", lambda e, pg=pg, s_=s_: e.activation(out=s_, in_=pg[:], func=AF.Silu), reads=rpg, writes=[rs_])
                    P.op("dve", lambda e, pu=pu, s_=s_, j=j, t2=t2: e.tensor_tensor(
                        out=act[:, j, t2 * 512:(t2 + 1) * 512], in0=pu[:], in1=s_, op=ALU.mult),
                        reads=rpu + [rs_], writes=[rAct[j][t2]])
                self.relA(2)
            for oc in range(KC):
                sd, rd = self.acqB()
                for t2 in range(2):
                    pf, rpf = self.bank()
                    self.mm8(pf, rpf, sd, rd, lambda k, t2=t2: (act[:, k, t2 * 512:(t2 + 1) * 512], rAct[k][t2]), nk=FC)
                    fa = f2[:, oc, t2 * 512:(t2 + 1) * 512]
                    if (oc + t2) % 2 == 0:
                        P.op("act", lambda e, pf=pf, fa=fa: e.activation(out=fa, in_=pf[:], func=AF.Copy), reads=rpf, writes=[rF2[oc][t2]])
                    else:
                        P.op("dve", lambda e, pf=pf, fa=fa: e.tensor_copy(fa, pf[:]), reads=rpf, writes=[rF2[oc][t2]])
                self.relB()
            self.postnorm_add(l, 24, lambda c, li: (f2[:, c, li * 512:(li + 1) * 512], rF2[c][li]), tts, sq, rstdb, tmpb)

    def build(self, do_mixer=True, do_ffn=True):
        with contextlib.ExitStack() as st:
            self.alloc(st)
            self.weight_lists()
            self.setup()
            for s in range(self.nseq):
                self.load_x(s)
                for l in range(self.nl):
                    self.mixer(l)
                    if self.stop is None or self.stop == "ffn":
                        self.ffn(l)
                self.store_x(s)
            fw = [k for k in self.P.dma_sems if k not in ("c", "cs") and not (isinstance(k, tuple) and k[0] == "x")]
            self.P.emit(final_waits=fw)
        return self.nc


def _pack_inputs(inp):
    f = lambda a: np.ascontiguousarray(np.asarray(a, dtype=np.float32))
    cols = _win_cols()
    w_in = f(inp["w_in"])
    win = np.stack([_chunk_w(w_in[l], cols) for l in range(L)]).reshape(L, NWIN, 128, KC * 128)
    c128 = lambda n: [np.arange(i * 128, (i + 1) * 128) for i in range(n // 128)]
    wout = np.stack([_chunk_w(f(inp["w_out"])[l], c128(D)) for l in range(L)]).reshape(L, KC, 128, KC * 128)
    wg = np.stack([_chunk_w(f(inp["w_gate"])[l], c128(FF)) for l in range(L)]).reshape(L, FC, 128, KC * 128)
    wu = np.stack([_chunk_w(f(inp["w_up"])[l], c128(FF)) for l in range(L)]).reshape(L, FC, 128, KC * 128)
    wd = np.stack([_chunk_w(f(inp["w_down"])[l], c128(D)) for l in range(L)]).reshape(L, KC, 128, FC * 128)
    pp = np.zeros((128, L * PP_PER_L), np.float32)
    gdnc = np.zeros((1, L * 128), np.float32)
    for l in range(L):
        b = l * PP_PER_L
        for off, key in ((0, "attn_pre_norm"), (8, "attn_post_norm"), (16, "ffn_pre_norm"), (24, "ffn_post_norm")):
            pp[:, b + off:b + off + 8] = f(inp[key])[l].reshape(8, 128).T
        cw = f(inp["conv_dw"])[l]
        for cc in range(2):
            pp[:, b + 32 + cc * 31:b + 32 + (cc + 1) * 31] = cw[:, cc * 128:(cc + 1) * 128].T
        pp[:, b + 94:b + 96] = f(inp["conv_dw_bias"])[l].reshape(2, 128).T
        pp[:, b + 96:b + 98] = f(inp["conv_norm_gain"])[l].reshape(2, 128).T
        pp[:, b + 98:b + 100] = f(inp["conv_norm_bias"])[l].reshape(2, 128).T
        gs = f(inp["gdn_short_conv"])[l]
        for xi in range(3):
            for h in range(4):
                ch = xi * 512 + h * 128
                pp[:, b + 100 + (xi * 4 + h) * 4:b + 100 + (xi * 4 + h) * 4 + 4] = gs[:, ch:ch + 128].T
        pp[:, b + 148] = f(inp["gdn_out_norm"])[l]
        gdnc[0, l * 128:l * 128 + 64] = np.tile(f(inp["gdn_a_log"])[l], 16)
        gdnc[0, l * 128 + 64:l * 128 + 128] = np.tile(f(inp["gdn_dt_bias"])[l], 16)
    cs, ms, cmat = _host_consts()
    return dict(win=win, wout=wout, wgate=wg, wup=wu, wdown=wd, pp=pp, gdnc=gdnc, cs=cs, ms=ms, cmat=cmat)


def kernel(**inputs):
    x = np.asarray(inputs["x"], dtype=np.float32)
    shared = _pack_inputs(inputs)
    b = Builder(SEQ_PER_CORE, L)
    nc = b.build()
    in_maps = []
    for c in range(NCORE):
        xs = x[c * SEQ_PER_CORE:(c + 1) * SEQ_PER_CORE]
        m = dict(shared)
        m["xT"] = np.ascontiguousarray(xs.transpose(0, 2, 1))
        in_maps.append(m)
    res = run_bass_kernel_spmd(nc, in_maps, core_ids=list(range(NCORE)))
    out = np.empty_like(x)
    for c in range(NCORE):
        out[c * SEQ_PER_CORE:(c + 1) * SEQ_PER_CORE] = res.results[c]["outT"].transpose(0, 2, 1)
    return out
```

```python
import contextlib
import math
import numpy as np
import concourse.bass as bass
import concourse.mybir as mybir
from concourse.bass_utils import run_bass_kernel_spmd

F32 = mybir.dt.float32
BF16 = mybir.dt.bfloat16
AF = mybir.ActivationFunctionType
ALU = mybir.AluOpType

D = 1024
S = 2048
L = 4
NCORE = 8
SEQ_PER_CORE = 4
FF = 2816
KC = 8
FC = 22
NWIN = 31
RMS_EPS = 1e-6
LN_EPS = 1e-5
L2_EPS = 1e-6
NEG = -30000.0
MS_OFF = 384
MS_W = 2048 + MS_OFF
PP_PER_L = 149
ENGS = ("pe", "act", "dve", "pool", "sp")


class Res:
    __slots__ = ("name", "w", "r")

    def __init__(self, name):
        self.name = name
        self.w = None
        self.r = {}


class Op:
    __slots__ = ("eng", "fn", "waits", "sig", "sigval", "dma", "idx")


class Prog:
    def __init__(self, nc):
        self.nc = nc
        self.ops = {e: [] for e in ENGS}
        self.seen = {e: {} for e in ENGS}
        self.dma_sems = {}

    def _need(self, op, ev):
        if ev is None:
            return
        kind, k, val = ev
        if kind == "e" and k == "pe" and op.eng == "pe":
            return
        key = (kind, k)
        if self.seen[op.eng].get(key, -1) >= val:
            return
        if val > op.waits.get(key, -1):
            op.waits[key] = val

    def op(self, eng, fn, reads=(), writes=(), dma_key=None):
        o = Op()
        o.eng = eng
        o.fn = fn
        o.waits = {}
        o.sig = False
        o.sigval = None
        o.dma = dma_key
        o.idx = len(self.ops[eng])
        for r in reads:
            self._need(o, r.w)
        for w in writes:
            self._need(o, w.w)
            for k2, v2 in w.r.items():
                self._need(o, (k2[0], k2[1], v2))
        for key, v in o.waits.items():
            self.seen[eng][key] = v
            if key[0] == "e":
                self.ops[key[1]][v].sig = True
        if dma_key is not None:
            ent = self.dma_sems.setdefault(dma_key, [None, 0])
            ent[1] += 16
            ev = ("d", dma_key, ent[1])
        else:
            ev = ("e", eng, o.idx)
        rk = (ev[0], ev[1])
        for r in reads:
            if r.r.get(rk, -1) < ev[2]:
                r.r[rk] = ev[2]
        for w in writes:
            w.w = ev
            w.r = {}
        self.ops[eng].append(o)
        return o

    def barrier(self, engines=("pe", "act", "dve", "sp")):
        last = {}
        for e in engines:
            i = len(self.ops[e]) - 1
            while i >= 0 and self.ops[e][i].dma is not None:
                i -= 1
            last[e] = i
        dm = {k: v[1] for k, v in self.dma_sems.items()}
        for e in engines:
            o = Op()
            o.eng = e
            o.fn = lambda eng: eng.nop()
            o.waits = {}
            o.sig = False
            o.sigval = None
            o.dma = None
            o.idx = len(self.ops[e])
            for b in engines:
                if last[b] >= 0:
                    self._need(o, ("e", b, last[b]))
            for k, v in dm.items():
                self._need(o, ("d", k, v))
            for key, v in o.waits.items():
                self.seen[e][key] = v
                if key[0] == "e":
                    self.ops[key[1]][v].sig = True
            self.ops[e].append(o)

    def emit(self, final_waits=()):
        nc = self.nc
        with contextlib.ExitStack() as st:
            esem = {e: st.enter_context(nc.semaphore("s_" + e)) for e in ENGS}
            for i, (k, ent) in enumerate(self.dma_sems.items()):
                ent[0] = st.enter_context(nc.semaphore("d%d" % i))
            for e in ENGS:
                c = 0
                for o in self.ops[e]:
                    if o.sig and o.dma is None:
                        c += 1
                        o.sigval = c
            block = st.enter_context(nc.Block())

            def run(ename):
                def body(eng):
                    for o in self.ops[ename]:
                        ws = []
                        for key, v in o.waits.items():
                            if key[0] == "e":
                                ws.append((esem[key[1]], self.ops[key[1]][v].sigval))
                            else:
                                ws.append((self.dma_sems[key[1]][0], v))
                        for (s, v) in ws[:-1]:
                            eng.wait_ge(s, v)
                        ins = o.fn(eng)
                        if ws:
                            ins._wait_ge(ws[-1][0], ws[-1][1])
                        if o.dma is not None:
                            ins.then_inc(self.dma_sems[o.dma][0], 16)
                        elif o.sig:
                            ins.then_inc(esem[ename], 1)
                    if ename == "sp":
                        for k in final_waits:
                            eng.wait_ge(self.dma_sems[k][0], self.dma_sems[k][1])
                return body

            block.tensor(run("pe"))
            block.scalar(run("act"))
            block.vector(run("dve"))
            block.gpsimd(run("pool"))
            block.sync(run("sp"))


def _win_cols():
    A = 256
    cols = []
    q0, k0, v0 = 0, A, 2 * A

    def swap64(base):
        idx = np.arange(128)
        hd = idx // 64
        d = idx % 64
        return base + hd * 64 + (d + 32) % 64

    for base in (q0, q0 + 128, k0, k0 + 128):
        cols.append(base + np.arange(128))
        cols.append(swap64(base))
    cols.append(v0 + np.arange(128))
    cols.append(v0 + 128 + np.arange(128))
    cu = 3 * A
    cols.append(cu + np.arange(128))
    cols.append(cu + 256 + np.arange(128))
    cols.append(cu + 128 + np.arange(128))
    cols.append(cu + 384 + np.arange(128))
    gq = 3 * A + 512
    gg = gq + 1536
    ga = gg + 512
    cols.append(np.concatenate([ga + np.arange(8), np.full(120, -1)]))
    for h in range(4):
        cols.append(gq + h * 128 + np.arange(128))
        cols.append(gq + 512 + h * 128 + np.arange(128))
        cols.append(gq + 1024 + h * 128 + np.arange(128))
        cols.append(gg + h * 128 + np.arange(128))
    return cols


def _chunk_w(w, ncols_list):
    K = w.shape[0]
    out = np.zeros((len(ncols_list), 128, K // 128, 128), np.float32)
    for i, cl in enumerate(ncols_list):
        cl = np.asarray(cl)
        ok = cl >= 0
        blk = np.zeros((K, 128), np.float32)
        blk[:, ok] = w[:, cl[ok]]
        out[i] = blk.reshape(K // 128, 128, 128).transpose(1, 0, 2)
    return out


def _host_consts():
    pos = np.arange(S, dtype=np.float32)
    half = 32
    inv_freq = np.exp(-math.log(10000.0) * np.arange(half, dtype=np.float32) * (2.0 / 64)).astype(np.float32)
    ang = pos[None, :] * inv_freq[:, None]
    cos = np.cos(ang).astype(np.float32)
    sin = np.sin(ang).astype(np.float32)
    d = np.arange(128) % 64
    cs = np.zeros((2, 128, S), np.float32)
    cs[0] = cos[d % 32]
    cs[1] = np.where((d < 32)[:, None], -sin[d % 32], sin[d % 32])
    delta = np.arange(MS_W)[None, :] - MS_OFF - np.arange(128)[:, None]
    c = ((delta >= 0) & (delta <= 128)).astype(np.float32)
    c += ((delta >= 0) & (delta <= 512) & (delta % 4 == 0))
    c += ((delta >= 0) & (delta <= 2048) & (delta % 16 == 0))
    ms = c.astype(np.float32)
    p = np.arange(128)[:, None]
    f = np.arange(128)[None, :]
    ident = (p == f).astype(np.float32)
    tri = (p <= f).astype(np.float32)
    nmls = np.where(p <= f, NEG, 0.0).astype(np.float32)
    nmui = np.where(f < p, NEG, 0.0).astype(np.float32)
    cmat = np.concatenate([ident, tri, nmls, nmui], axis=1).astype(np.float32)
    return cs, ms, cmat


class Builder:
    def __init__(self, nseq, nlayers, taps=(), stop=None):
        self.stop = stop
        self.nseq = nseq
        self.nl = nlayers
        self.taps = set(taps)
        self.tap_out = {}
        nc = bass.Bass("TRN2", target_bir_lowering=False)
        self.nc = nc
        dt = nc.dram_tensor
        self.d_x = dt("xT", [nseq, D, S], F32, kind="ExternalInput").ap()
        self.d_win = dt("win", [L, NWIN, 128, KC * 128], F32, kind="ExternalInput").ap()
        self.d_wout = dt("wout", [L, KC, 128, KC * 128], F32, kind="ExternalInput").ap()
        self.d_wg = dt("wgate", [L, FC, 128, KC * 128], F32, kind="ExternalInput").ap()
        self.d_wu = dt("wup", [L, FC, 128, KC * 128], F32, kind="ExternalInput").ap()
        self.d_wd = dt("wdown", [L, KC, 128, FC * 128], F32, kind="ExternalInput").ap()
        self.d_pp = dt("pp", [128, L * PP_PER_L], F32, kind="ExternalInput").ap()
        self.d_gdnc = dt("gdnc", [1, L * 128], F32, kind="ExternalInput").ap()
        self.d_cs = dt("cs", [2, 128, S], F32, kind="ExternalInput").ap()
        self.d_ms = dt("ms", [128, MS_W], F32, kind="ExternalInput").ap()
        self.d_cmat = dt("cmat", [128, 512], F32, kind="ExternalInput").ap()
        self.d_out = dt("outT", [nseq, D, S], F32, kind="ExternalOutput").ap()
        self.P = Prog(nc)
        self.RA = 6
        self.RB = 2

    def alloc(self, st):
        nc = self.nc

        def sb(name, shape, dtp):
            return st.enter_context(nc.sbuf_tensor(name, shape, dtp))

        self.xT = sb("xT_sb", [128, KC, S], F32)
        self.rX = [[Res("x%d_%d" % (c, t)) for t in range(4)] for c in range(KC)]
        self.cmat = sb("cmat_sb", [128, 512], F32)
        self.ident_f = self.cmat[:, 0:128]
        self.tri_f = self.cmat[:, 128:256]
        self.nmls = self.cmat[:, 256:384]
        self.nmui = self.cmat[:, 384:512]
        self.ident_b = sb("ident_b", [128, 128], BF16)
        self.ones_b = sb("ones_b", [128, 4, 128], BF16)
        self.ones_f = sb("ones_f", [128, 128], F32)
        self.pp = sb("pp_sb", [128, L * PP_PER_L], F32)
        self.gdnc = sb("gdnc_sb", [128, L * 128], F32)
        self.rC = Res("consts")
        self.ringA = [sb("ra%d" % i, [128, KC, 128], BF16) for i in range(self.RA)]
        self.rA = [Res("ra%d" % i) for i in range(self.RA)]
        self.ringB = [sb("rb%d" % i, [128, FC, 128], BF16) for i in range(self.RB)]
        self.rB = [Res("rb%d" % i) for i in range(self.RB)]
        self.ARW = 28672
        self.arena = sb("arena", [128, self.ARW], F32)
        self.banks = [st.enter_context(nc.psum_tensor("ps%d" % i, [128, 512], F32)) for i in range(8)]
        self.rbank = [[Res("ps%d" % i)] for i in range(8)]
        self.banks_b = [b.bitcast(BF16) for b in self.banks]
        self.bank_i = 0
        self.held = set()

    def view(self, off_words, nwords, dtp=F32, shape=None):
        ap = self.arena[:, off_words:off_words + nwords]
        if dtp is not F32:
            ap = ap.bitcast(dtp)
        if shape is not None and len(shape) == 2:
            ap = ap.rearrange("p (a b) -> p a b", a=shape[0], b=shape[1])
        elif shape is not None and len(shape) == 3:
            ap = ap.rearrange("p (a b c) -> p a b c", a=shape[0], b=shape[1], c=shape[2])
        return ap

    def _next_bank(self, hold):
        while self.bank_i in self.held:
            self.bank_i = (self.bank_i + 1) % 8
        i = self.bank_i
        self.bank_i = (self.bank_i + 1) % 8
        if hold:
            self.held.add(i)
        return i

    def bank(self, hold=False):
        i = self._next_bank(hold)
        return self.banks[i], self.rbank[i]

    def bankb(self, hold=False):
        i = self._next_bank(hold)
        return self.banks_b[i], self.rbank[i]

    def unhold(self, rps):
        self.held.discard(int(rps[0].name[2:]))

    def weight_lists(self):
        la, lb = [], []
        for s in range(self.nseq):
            for l in range(self.nl):
                for rep in range(2):
                    for ci in range(8):
                        la.append(self.d_win[l, ci])
                for ci in range(8, NWIN):
                    la.append(self.d_win[l, ci])
                for oc in range(KC):
                    la.append(self.d_wout[l, oc])
                for half in range(2):
                    for j in range(FC):
                        la.append(self.d_wg[l, j])
                        la.append(self.d_wu[l, j])
                    for oc in range(KC):
                        lb.append(self.d_wd[l, oc])
        self.listA, self.listB = la, lb
        self.issA = self.issB = 0
        self.conA = self.conB = 0
        self.doneA = self.doneB = 0

    def _pump(self, which):
        P = self.P
        if which == "A":
            while self.issA < len(self.listA) and self.issA - self.RA < self.doneA:
                m = self.issA
                slot = self.ringA[m % self.RA]
                src = self.listA[m]
                P.op("pool", lambda e, slot=slot, src=src: e.dma_start(
                    out=slot[:].rearrange("p k n -> p (k n)"), in_=src),
                    writes=[self.rA[m % self.RA]], dma_key=("A", m % self.RA))
                self.issA += 1
        else:
            while self.issB < len(self.listB) and self.issB - self.RB < self.doneB:
                m = self.issB
                slot = self.ringB[m % self.RB]
                src = self.listB[m]
                P.op("pool", lambda e, slot=slot, src=src: e.dma_start(
                    out=slot[:].rearrange("p k n -> p (k n)"), in_=src),
                    writes=[self.rB[m % self.RB]], dma_key=("B", m % self.RB))
                self.issB += 1

    def acqA(self):
        self._pump("A")
        n = self.conA
        assert n < self.issA, "ring A deadlock"
        self.conA += 1
        return self.ringA[n % self.RA], self.rA[n % self.RA]

    def relA(self, k=1):
        self.doneA += k
        self._pump("A")

    def acqB(self):
        self._pump("B")
        n = self.conB
        assert n < self.issB, "ring B deadlock"
        self.conB += 1
        return self.ringB[n % self.RB], self.rB[n % self.RB]

    def relB(self):
        self.doneB += 1
        self._pump("B")

    def tap(self, name, ap, shape, res, bf=False):
        if name not in self.taps:
            return
        d = self.nc.dram_tensor("tap_" + name, list(shape), F32, kind="ExternalOutput").ap()
        self.tap_out[name] = d
        q = "pool" if bf else "sp"
        self.P.op(q, lambda e: e.dma_start(out=d, in_=ap), reads=list(res), dma_key=("tap_" + q + ("_" + name if bf else "")))

    def mm8(self, ps, rps, slot, rslot, rhs_fn, nk=KC, col=slice(None)):
        P = self.P
        for k in range(nk):
            rhs, rr = rhs_fn(k)
            P.op("pe", lambda e, k=k, rhs=rhs: e.matmul(ps[:], slot[:, k, col], rhs, start=(k == 0), stop=(k == nk - 1)),
                 reads=[rslot, rr], writes=rps)

    def stats_rstd(self, srcs, ones_idx, eps, rstd, rrstd, sq, rsq):
        P = self.P
        ps, rps = self.bank()
        W = 512
        n = len(srcs)
        for c, (src, rs) in enumerate(srcs):
            sqa, sqr = sq[c % len(sq)]
            P.op("act", lambda e, src=src, sqa=sqa: e.activation(out=sqa, in_=src, func=AF.Square),
                 reads=[rs], writes=[sqr])
            P.op("pe", lambda e, c=c, sqa=sqa: e.matmul(ps[:, 0:W], self.ones_b[:, ones_idx, :], sqa,
                                                       start=(c == 0), stop=(c == n - 1)),
                 reads=[sqr, self.rC], writes=rps)
        P.op("act", lambda e: e.activation(out=rstd, in_=ps[:, 0:W], func=AF.Sqrt, bias=float(eps), scale=1.0),
             reads=rps, writes=[rrstd])
        P.op("dve", lambda e: e.reciprocal(rstd, rstd), reads=[rrstd], writes=[rrstd])

    def setup(self):
        P = self.P
        nc = self.nc
        P.op("sp", lambda e: e.dma_start(out=self.cmat[:], in_=self.d_cmat), writes=[self.rC], dma_key="c")
        P.op("sp", lambda e: e.dma_start(out=self.pp[:], in_=self.d_pp), writes=[self.rC], dma_key="c")
        P.op("sp", lambda e: e.dma_start(out=self.gdnc[:], in_=self.d_gdnc.partition_broadcast(128)),
             writes=[self.rC], dma_key="c")
        P.op("dve", lambda e: e.tensor_copy(self.ident_b[:], self.ident_f), reads=[self.rC], writes=[self.rC])
        P.op("dve", lambda e: e.memset(self.ones_b[:, 0, :], 1.0), writes=[self.rC])
        P.op("dve", lambda e: e.memset(self.ones_b[:, 1, :], 1.0 / 1024), writes=[self.rC])
        P.op("dve", lambda e: e.memset(self.ones_b[:, 2, :], 1.0 / 256), writes=[self.rC])
        P.op("dve", lambda e: e.memset(self.ones_b[:, 3, :], 1.0 / 128), writes=[self.rC])
        P.op("dve", lambda e: e.memset(self.ones_f[:], 1.0), writes=[self.rC])

    def ppcol(self, l, off, n=1):
        b = l * PP_PER_L + off
        return self.pp[:, b:b + n]


    def load_x(self, s):
        P = self.P
        for c in range(KC):
            P.op("sp", lambda e, c=c: e.dma_start(out=self.xT[:, c, :], in_=self.d_x[s, c * 128:(c + 1) * 128, :]),
                 writes=self.rX[c], dma_key=("x", c))

    def store_x(self, s):
        P = self.P
        for c in range(KC):
            P.op("sp", lambda e, c=c: e.dma_start(out=self.d_out[s, c * 128:(c + 1) * 128, :], in_=self.xT[:, c, :]),
                 reads=self.rX[c], dma_key=("o", c))

    def prenorm(self, l, gain_off, hT, rH, tts, sq, rstd_bufs):
        P = self.P
        for li, tt in enumerate(tts):
            tok = slice(tt * 512, (tt + 1) * 512)
            rstd, rr = rstd_bufs[li % len(rstd_bufs)]
            self.stats_rstd([(self.xT[:, c, tok], self.rX[c][tt]) for c in range(KC)], 1, RMS_EPS, rstd, rr, sq, None)
            for c in range(KC):
                P.op("dve", lambda e, c=c, tok=tok, li=li, rstd=rstd: e.scalar_tensor_tensor(
                    out=hT[:, c, li * 512:(li + 1) * 512], in0=self.xT[:, c, tok], scalar=self.ppcol(l, gain_off + c),
                    in1=rstd, op0=ALU.mult, op1=ALU.mult),
                    reads=[self.rX[c][tt], rr, self.rC], writes=[rH[c][li]])

    def postnorm_add(self, l, gain_off, f_fn, tts, sq, rstd_bufs, tmp_bufs):
        P = self.P
        for li, tt in enumerate(tts):
            tok = slice(tt * 512, (tt + 1) * 512)
            rstd, rr = rstd_bufs[li % len(rstd_bufs)]
            self.stats_rstd([f_fn(c, li) for c in range(KC)], 1, RMS_EPS, rstd, rr, sq, None)
            for c in range(KC):
                fa, fr = f_fn(c, li)
                tmp, rt = tmp_bufs[c % len(tmp_bufs)]
                P.op("dve", lambda e, c=c, fa=fa, tmp=tmp, rstd=rstd: e.scalar_tensor_tensor(
                    out=tmp, in0=fa, scalar=self.ppcol(l, gain_off + c), in1=rstd, op0=ALU.mult, op1=ALU.mult),
                    reads=[fr, rr, self.rC], writes=[rt])
                P.op("dve", lambda e, c=c, tok=tok, tmp=tmp: e.tensor_tensor(
                    out=self.xT[:, c, tok], in0=self.xT[:, c, tok], in1=tmp, op=ALU.add),
                    reads=[rt, self.rX[c][tt]], writes=[self.rX[c][tt]])

    def mixer(self, l):
        P = self.P
        HOFF, MOFF, SOFF = 0, 8192, 16384
        hT = self.view(HOFF, 8192, BF16, (KC, S))
        mixT = self.view(MOFF, 8192, BF16, (KC, S))
        rH = [[Res("h%d_%d" % (c, t)) for t in range(4)] for c in range(KC)]
        rM = [[Res("m%d_%d" % (c, t)) for t in range(4)] for c in range(KC)]
        self.hT, self.rH, self.mixT, self.rM = hT, rH, mixT, rM

        def hrhs(tt):
            return lambda k: (hT[:, k, tt * 512:(tt + 1) * 512], rH[k][tt])

        so = SOFF
        sq = [(self.view(so + i * 256, 256, BF16), Res("sq%d" % i)) for i in range(2)]
        rstdb = [(self.view(so + 512 + i * 512, 512), Res("rstd%d" % i)) for i in range(2)]
        P.barrier()
        self.prenorm(l, 0, hT, rH, [0, 1, 2, 3], sq, rstdb)
        if self.stop == "pre":
            return
        self.tap("hT", hT.rearrange("p c t -> p (c t)"), [128, KC * S], [r for rr in rH for r in rr], bf=True)

        P.barrier()
        csb = self.view(SOFF, 2048, F32, (2, 1024))
        rcs = Res("cs")
        t1 = [(self.view(SOFF + 2048 + i * 512, 512), Res("t1_%d" % i)) for i in range(2)]
        QK = self.view(SOFF + 3072, 4096, BF16, (4, S))
        rQK = [[Res("qk%d_%d" % (i, t)) for t in range(4)] for i in range(4)]
        for half in range(2):
            P.op("sp", lambda e, half=half: e.dma_start(out=csb[:, 0, :], in_=self.d_cs[0, :, half * 1024:(half + 1) * 1024]),
                 writes=[rcs], dma_key="cs")
            P.op("sp", lambda e, half=half: e.dma_start(out=csb[:, 1, :], in_=self.d_cs[1, :, half * 1024:(half + 1) * 1024]),
                 writes=[rcs], dma_key="cs")
            for i in range(4):
                sa, ra = self.acqA()
                sbw, rb = self.acqA()
                for t2 in range(2):
                    tt = half * 2 + t2
                    pa, rpa = self.bank()
                    pb, rpb = self.bank()
                    self.mm8(pa, rpa, sa, ra, hrhs(tt))
                    self.mm8(pb, rpb, sbw, rb, hrhs(tt))
                    (ta, rta), (tb, rtb) = t1
                    lc = slice(t2 * 512, (t2 + 1) * 512)
                    P.op("dve", lambda e, pa=pa, ta=ta, lc=lc: e.tensor_tensor(out=ta, in0=pa[:], in1=csb[:, 0, lc], op=ALU.mult),
                         reads=rpa + [rcs], writes=[rta])
                    P.op("dve", lambda e, pb=pb, tb=tb, lc=lc: e.tensor_tensor(out=tb, in0=pb[:], in1=csb[:, 1, lc], op=ALU.mult),
                         reads=rpb + [rcs], writes=[rtb])
                    P.op("dve", lambda e, i=i, tt=tt, ta=ta, tb=tb: e.tensor_tensor(
                        out=QK[:, i, tt * 512:(tt + 1) * 512], in0=ta, in1=tb, op=ALU.add),
                        reads=[rta, rtb], writes=[rQK[i][tt]])
                self.relA(2)
        if self.stop == "rot":
            return
        self.tap("QK", QK.rearrange("p c t -> p (c t)"), [128, 4 * S], [r for rr in rQK for r in rr], bf=True)

        P.barrier()
        VW = 16 * 4 * 66
        Vp = self.view(SOFF, VW // 2, BF16, (16, 4, 66))
        rV = [Res("v%d" % t) for t in range(16)]
        rV1 = Res("vones")
        P.op("dve", lambda e: e.memset(Vp[:, :, :, 64:65], 1.0), writes=[rV1])
        s0, r0 = self.acqA()
        s1, r1 = self.acqA()
        for t16 in range(16):
            pv, rpv = self.bank()
            for vc, (sv, rv) in enumerate(((s0, r0), (s1, r1))):
                for k in range(KC):
                    P.op("pe", lambda e, k=k, vc=vc, sv=sv, pv=pv, t16=t16: e.matmul(
                        pv[:, vc * 128:(vc + 1) * 128], hT[:, k, t16 * 128:(t16 + 1) * 128], sv[:, k, :],
                        start=(k == 0), stop=(k == KC - 1)),
                        reads=[rv, rH[k][t16 // 4]], writes=rpv)
            P.op("act", lambda e, pv=pv, t16=t16: e.activation(
                out=Vp[:, t16, :, 0:64], in_=pv[:, 0:256].rearrange("p (h d) -> p h d", h=4, d=64), func=AF.Copy),
                reads=rpv, writes=[rV[t16]])
        self.relA(2)

        if self.stop == "v":
            return
        msb = self.view(SOFF + 8704, MS_W // 2, BF16)
        rms = Res("ms")
        PB = [(self.view(SOFF + 7168 + i * 256, 256, BF16), Res("pb%d" % i)) for i in range(3)]
        atm = self.view(SOFF + 7168 + 768, 512, BF16, (4, 256))
        ratm = Res("atm")
        rden = self.view(SOFF + 7168 + 1280, 4)
        rrden = Res("rden")
        stg = self.view(MOFF, MS_W)
        rstg = Res("stg")
        P.op("sp", lambda e: e.dma_start(out=stg, in_=self.d_ms), writes=[rstg], dma_key="cs")
        P.op("act", lambda e: e.activation(out=msb, in_=stg, func=AF.Copy), reads=[rstg], writes=[rms])
        P.barrier()
        for Q in range(4):
            qs_tok = slice(Q * 512, (Q + 1) * 512)
            for h in range(4):
                cq = h // 2
                base = 64 * (h % 2)
                po, rpo = self.bank(hold=True)
                pov = po[:, 0:264].rearrange("p (a b) -> p a b", a=4, b=66)
                nkt = 4 * Q + 4
                def smm(kt, cq=cq, base=base, qs_tok=qs_tok, Q=Q):
                    pS, rpS = self.bank()
                    P.op("pe", lambda e, pS=pS, cq=cq, base=base, kt=kt, qs_tok=qs_tok: e.matmul(
                        pS[:], QK[base:base + 64, 2 + cq, kt * 128:(kt + 1) * 128], QK[base:base + 64, cq, qs_tok],
                        start=True, stop=True),
                        reads=[rQK[2 + cq][kt // 4], rQK[cq][Q]], writes=rpS)
                    return pS, rpS

                nxtS = smm(0)
                for kt in range(nkt):
                    pS, rpS = nxtS
                    if kt + 1 < nkt:
                        nxtS = smm(kt + 1)
                    pb, rpb = PB[(kt) % 3]
                    P.op("act", lambda e, pS=pS, pb=pb: e.activation(out=pb, in_=pS[:], func=AF.Exp, scale=0.125),
                         reads=rpS, writes=[rpb])
                    off = Q * 512 - kt * 128 + MS_OFF
                    P.op("dve", lambda e, pb=pb, off=off: e.tensor_tensor(out=pb, in0=pb, in1=msb[:, off:off + 512], op=ALU.mult),
                         reads=[rpb, rms], writes=[rpb])
                    for qs in range(4):
                        last = 4 * Q + qs
                        if kt > last:
                            continue
                        P.op("pe", lambda e, pov=pov, qs=qs, pb=pb, kt=kt, h=h, last=last: e.matmul(
                            pov[:, qs, 0:65], pb[:, qs * 128:(qs + 1) * 128], Vp[:, kt, h, 0:65],
                            start=(kt == 0 and qs == 0), stop=(kt == last), skip_group_check=True),
                            reads=[rpb, rV[kt], rV1], writes=rpo)
                P.op("dve", lambda e, pov=pov: e.reciprocal(rden.unsqueeze(2), pov[:, :, 64:65]),
                     reads=rpo, writes=[rrden])
                P.op("dve", lambda e, pov=pov, h=h: e.tensor_tensor(
                    out=atm[:, :, h * 64:(h + 1) * 64], in0=pov[:, :, 0:64],
                    in1=rden.unsqueeze(2).to_broadcast([128, 4, 64]), op=ALU.mult),
                    reads=rpo + [rrden], writes=[ratm])
                self.unhold(rpo)
            for c2 in range(2):
                pt, rpt = self.bankb()
                for qs in range(4):
                    P.op("pe", lambda e, qs=qs, c2=c2, pt=pt: e.transpose(
                        pt[:, qs * 128:(qs + 1) * 128], atm[:, qs, c2 * 128:(c2 + 1) * 128], self.ident_b[:]),
                        reads=[ratm, self.rC], writes=rpt)
                P.op("act", lambda e, c2=c2, qs_tok=qs_tok, pt=pt: e.activation(out=mixT[:, c2, qs_tok], in_=pt[:, 0:512], func=AF.Copy),
                     reads=rpt, writes=[rM[c2][Q]])
        if self.stop == "attn":
            return
        self.tap("mix_attn", mixT[:, 0:2, :].rearrange("p c t -> p (c t)"), [128, 2 * S], rM[0] + rM[1], bf=True)

        P.barrier()
        YW = 2080
        ybf = self.view(SOFF, YW, BF16, (2, YW))
        rY = [[Res("y%d_%d" % (c, t)) for t in range(4)] for c in range(2)]
        rYp = Res("ypad")
        dg = self.view(SOFF + 2080, 3968, BF16, (2, 31, 128))
        rdg = Res("dg")
        yc = self.view(SOFF + 6048, 4096, F32, (2, S))
        rYc = [[Res("yc%d_%d" % (c, t)) for t in range(4)] for c in range(2)]
        ctmp = [(self.view(SOFF + 10144 + i * 512, 512), Res("ctmp%d" % i)) for i in range(2)]
        csq = [(self.view(SOFF + 11168 + i * 256, 256, BF16), Res("csq%d" % i)) for i in range(4)]
        P.op("dve", lambda e: e.memset(ybf[:, :, 0:30], 0.0), writes=[rYp])
        for cc in range(2):
            P.op("dve", lambda e, cc=cc: e.tensor_tensor(
                out=dg[:, cc, :, :], in0=self.ident_b[:].unsqueeze(1).to_broadcast([128, 31, 128]),
                in1=self.ppcol(l, 32 + cc * 31, 31).unsqueeze(2).to_broadcast([128, 31, 128]), op=ALU.mult),
                reads=[self.rC], writes=[rdg])
        for cc in range(2):
            sv, rv = self.acqA()
            sg, rg = self.acqA()
            for tt in range(4):
                pv, rpv = self.bank()
                pg, rpg = self.bank()
                self.mm8(pv, rpv, sv, rv, hrhs(tt))
                self.mm8(pg, rpg, sg, rg, hrhs(tt))
                tmp, rt = ctmp[tt % 2]
                P.op("act", lambda e, pg=pg, tmp=tmp: e.activation(out=tmp, in_=pg[:], func=AF.Sigmoid), reads=rpg, writes=[rt])
                P.op("dve", lambda e, pv=pv, tmp=tmp, cc=cc, tt=tt: e.tensor_tensor(
                    out=ybf[:, cc, 30 + tt * 512:30 + (tt + 1) * 512], in0=pv[:], in1=tmp, op=ALU.mult),
                    reads=rpv + [rt], writes=[rY[cc][tt]])
            self.relA(2)
        for cc in range(2):
            for tt in range(4):
                pc, rpc = self.bank()
                rd = [rY[cc][tt], rdg, rYp] + ([rY[cc][tt - 1]] if tt > 0 else [])
                for j in range(31):
                    P.op("pe", lambda e, pc=pc, cc=cc, tt=tt, j=j: e.matmul(
                        pc[:], dg[:, cc, j, :], ybf[:, cc, tt * 512 + j:tt * 512 + j + 512], start=(j == 0), stop=(j == 30)),
                        reads=rd, writes=rpc)
                P.op("act", lambda e, pc=pc, cc=cc, tt=tt: e.activation(
                    out=yc[:, cc, tt * 512:(tt + 1) * 512], in_=pc[:], func=AF.Identity, bias=self.ppcol(l, 94 + cc), scale=1.0),
                    reads=rpc + [self.rC], writes=[rYc[cc][tt]])
        for tt in range(4):
            tok = slice(tt * 512, (tt + 1) * 512)
            pm, rpm = self.bank()
            pq, rpq = self.bank()
            for cc in range(2):
                yb, ryb = csq[cc]
                ys, rys = csq[2 + cc]
                P.op("dve", lambda e, yb=yb, cc=cc, tok=tok: e.tensor_copy(yb, yc[:, cc, tok]), reads=[rYc[cc][tt]], writes=[ryb])
                P.op("act", lambda e, ys=ys, cc=cc, tok=tok: e.activation(out=ys, in_=yc[:, cc, tok], func=AF.Square),
                     reads=[rYc[cc][tt]], writes=[rys])
                P.op("pe", lambda e, pm=pm, yb=yb, cc=cc: e.matmul(pm[:], self.ones_b[:, 2, :], yb, start=(cc == 0), stop=(cc == 1)),
                     reads=[ryb, self.rC], writes=rpm)
                P.op("pe", lambda e, pq=pq, ys=ys, cc=cc: e.matmul(pq[:], self.ones_b[:, 2, :], ys, start=(cc == 0), stop=(cc == 1)),
                     reads=[rys, self.rC], writes=rpq)
            (ta, rta), (tb, rtb) = ctmp
            P.op("act", lambda e, pm=pm, ta=ta: e.activation(out=ta, in_=pm[:], func=AF.Square), reads=rpm, writes=[rta])
            P.op("dve", lambda e, pq=pq, ta=ta: e.tensor_tensor(out=ta, in0=pq[:], in1=ta, op=ALU.subtract), reads=rpq + [rta], writes=[rta])
            P.op("act", lambda e, ta=ta: e.activation(out=ta, in_=ta, func=AF.Sqrt, bias=float(LN_EPS), scale=1.0), reads=[rta], writes=[rta])
            P.op("dve", lambda e, ta=ta: e.reciprocal(ta, ta), reads=[rta], writes=[rta])
            for cc in range(2):
                P.op("dve", lambda e, pm=pm, tb=tb, cc=cc, tok=tok: e.tensor_tensor(out=tb, in0=yc[:, cc, tok], in1=pm[:], op=ALU.subtract),
                     reads=rpm + [rYc[cc][tt]], writes=[rtb])
                P.op("dve", lambda e, ta=ta, tb=tb: e.tensor_tensor(out=tb, in0=tb, in1=ta, op=ALU.mult), reads=[rta, rtb], writes=[rtb])
                P.op("act", lambda e, tb=tb, cc=cc, tok=tok: e.activation(
                    out=mixT[:, 2 + cc, tok], in_=tb, func=AF.Silu, bias=self.ppcol(l, 98 + cc), scale=self.ppcol(l, 96 + cc)),
                    reads=[rtb, self.rC], writes=[rM[2 + cc][tt]])
        if self.stop == "conv":
            return
        self.tap("mix_conv", mixT[:, 2:4, :].rearrange("p c t -> p (c t)"), [128, 2 * S], rM[2] + rM[3], bf=True)

        P.barrier()
        self.gdn(l, SOFF)
        if self.stop == "gdn":
            return
        self.tap("mix_gdn", mixT[:, 4:8, :].rearrange("p c t -> p (c t)"), [128, 4 * S], rM[4] + rM[5] + rM[6] + rM[7], bf=True)

        P.barrier()
        fA = self.view(HOFF, 8192, F32, (4, S))
        fB = self.view(SOFF, 8192, F32, (4, S))
        rF = [[Res("f%d_%d" % (c, t)) for t in range(4)] for c in range(KC)]

        def fap(c, tt):
            t = fA if c < 4 else fB
            return t[:, c % 4, tt * 512:(tt + 1) * 512], rF[c][tt]

        so = SOFF + 8192
        sq = [(self.view(so + i * 256, 256, BF16), Res("wsq%d" % i)) for i in range(2)]
        rstdb = [(self.view(so + 512 + i * 512, 512), Res("wrstd%d" % i)) for i in range(2)]
        tmpb = [(self.view(so + 1536 + i * 512, 512), Res("wtmp%d" % i)) for i in range(2)]
        for oc in range(KC):
            sw, rw = self.acqA()
            for tt in range(4):
                pf, rpf = self.bank()
                self.mm8(pf, rpf, sw, rw, lambda k, tt=tt: (mixT[:, k, tt * 512:(tt + 1) * 512], rM[k][tt]))
                fa, fr = fap(oc, tt)
                if (oc + tt) % 2 == 0:
                    P.op("act", lambda e, pf=pf, fa=fa: e.activation(out=fa, in_=pf[:], func=AF.Copy), reads=rpf, writes=[fr])
                else:
                    P.op("dve", lambda e, pf=pf, fa=fa: e.tensor_copy(fa, pf[:]), reads=rpf, writes=[fr])
            self.relA()
        self.postnorm_add(l, 8, fap, [0, 1, 2, 3], sq, rstdb, tmpb)
        self.tap("x_mid", self.xT[:].rearrange("p c t -> p (c t)"), [128, KC * S], [r for rr in self.rX for r in rr])

    def gdn(self, l, SOFF):
        P = self.P
        hT, rH, mixT, rM = self.hT, self.rH, self.mixT, self.rM
        o = SOFF
        zbuf = self.view(o, 2052)
        o += 2052
        acc = self.view(o, 2048)
        o += 2048
        qn = self.view(o, 1024, BF16)
        o += 1024
        kn = self.view(o, 1024, BF16)
        o += 1024
        vT = self.view(o, 1024, BF16)
        o += 1024
        sgT = self.view(o, 1024, BF16)
        o += 1024
        small = o
        rz = [Res("z%d" % t) for t in range(4)]
        rzp = Res("zpad")
        racc = [Res("acc%d" % t) for t in range(4)]
        rqn = [Res("qn%d" % t) for t in range(4)]
        rkn = [Res("kn%d" % t) for t in range(4)]
        rvT = [Res("vT%d" % t) for t in range(4)]
        rsg = [Res("sg%d" % t) for t in range(4)]

        cnt = [small]

        def sm(nwords, dtp=F32, shape=None, name="t"):
            ap = self.view(cnt[0], nwords, dtp, shape)
            cnt[0] += nwords
            return ap, Res(name)

        ab, rab = sm(128, F32, None, "ab")
        abv = ab.rearrange("p (c e) -> p c e", c=16, e=8)
        xg, rxg = sm(64, name="xg")
        t64a, rt64a = sm(64, name="t64a")
        t64b, rt64b = sm(64, name="t64b")
        g_, rg_ = sm(64, name="g")
        beta, rbeta = sm(64, name="beta")
        nbeta, rnbeta = sm(64, name="nbeta")
        gc, rgc = sm(64, name="gc")
        ngc, rngc = sm(64, name="ngc")
        bg, rbg = sm(64, name="bg")
        kds, rkds = sm(64, name="kds")
        gle, rgle = sm(64, name="gle")
        sqb = [sm(256, BF16, None, "gsq%d" % i) for i in range(1)]
        rinv = [sm(512, F32, None, "rinv%d" % i) for i in range(1)]
        dgc, rdgc = sm(128, name="dgc")
        tl, rtl = sm(128, name="tl")
        tu, rtu = tl, rtl
        El, rEl = sm(128, name="El")
        Eu, rEu = sm(128, name="Eu")
        gamr, rgamr = sm(128, name="gamr")
        knf, rknf = sm(128, name="knf")
        kd, rkd = sm(64, BF16, None, "kd")
        vtm, rvtm = sm(64, BF16, None, "vtm")
        rr_, rrr = sm(128, name="r")
        gam, rgam = sm(64, name="gam")
        Ab = [sm(128, F32, None, "A%d" % i) for i in range(2)]
        Bb = [sm(128, F32, None, "B%d" % i) for i in range(2)]
        TTb = [sm(128, F32, None, "TT%d" % i) for i in range(2)]
        intraT, rintra = sm(64, BF16, None, "intraT")
        qg, rqg = sm(64, BF16, None, "qg")
        vnew, rvnew = sm(64, BF16, None, "vnew")
        Sf, rSf = sm(128, name="Sf")
        Sb, rSb = sm(64, BF16, None, "Sb")
        ss, rss = sm(64, name="ss")
        onb, ronb = sm(64, BF16, None, "on")
        junk, rjunk = tl, rtl
        assert cnt[0] <= SOFF + 12288, cnt[0]

        def hrhs(tt):
            return lambda k: (hT[:, k, tt * 512:(tt + 1) * 512], rH[k][tt])

        gview = self.gdnc[:, l * 128:(l + 1) * 128]
        alog_t = gview[:, 0:64]
        dtb_t = gview[:, 64:128]

        sab, rsab = self.acqA()
        pab, rpab = self.bank()
        for c16 in range(16):
            for k in range(KC):
                P.op("pe", lambda e, k=k, c16=c16: e.matmul(
                    pab[:, c16 * 8:(c16 + 1) * 8], hT[:, k, c16 * 128:(c16 + 1) * 128], sab[:, k, 0:8],
                    start=(k == 0), stop=(k == KC - 1)),
                    reads=[rsab, rH[k][c16 // 4]], writes=rpab)
        self.relA()
        P.op("act", lambda e: e.activation(out=ab, in_=pab[:, 0:128], func=AF.Copy), reads=rpab, writes=[rab])
        v3 = lambda t: t.rearrange("p (c h) -> p c h", c=16, h=4)
        P.op("dve", lambda e: e.tensor_tensor(out=v3(xg), in0=abv[:, :, 0:4], in1=v3(dtb_t), op=ALU.add), reads=[rab, self.rC], writes=[rxg])
        P.op("act", lambda e: e.activation(out=t64a, in_=xg, func=AF.Abs), reads=[rxg], writes=[rt64a])
        P.op("act", lambda e: e.activation(out=t64a, in_=t64a, func=AF.Exp, scale=-1.0), reads=[rt64a], writes=[rt64a])
        P.op("act", lambda e: e.activation(out=t64a, in_=t64a, func=AF.Ln, bias=1.0, scale=1.0), reads=[rt64a], writes=[rt64a])
        P.op("dve", lambda e: e.tensor_scalar_max(out=t64b, in0=xg, scalar1=0.0), reads=[rxg], writes=[rt64b])
        P.op("dve", lambda e: e.tensor_tensor(out=t64b, in0=t64b, in1=t64a, op=ALU.add), reads=[rt64a, rt64b], writes=[rt64b])
        P.op("act", lambda e: e.activation(out=t64a, in_=alog_t, func=AF.Exp), reads=[self.rC, rt64a], writes=[rt64a])
        P.op("dve", lambda e: e.scalar_tensor_tensor(out=g_, in0=t64b, scalar=-1.0, in1=t64a, op0=ALU.mult, op1=ALU.mult),
             reads=[rt64a, rt64b], writes=[rg_])
        P.op("act", lambda e: e.activation(out=v3(beta), in_=abv[:, :, 4:8], func=AF.Sigmoid), reads=[rab], writes=[rbeta])
        P.op("dve", lambda e: e.tensor_scalar_mul(out=nbeta, in0=beta, scalar1=-1.0), reads=[rbeta], writes=[rnbeta])
        pg, rpg0 = self.bank()
        pg2, rpg1 = self.bank()
        P.op("pe", lambda e: e.matmul(pg[:, 0:64], self.tri_f, g_, start=True, stop=True), reads=[rg_, self.rC], writes=rpg0)
        P.op("pe", lambda e: e.matmul(pg2[:, 128:192], self.ones_f[:], g_, start=True, stop=True), reads=[rg_, self.rC], writes=rpg1)
        P.op("act", lambda e: e.activation(out=gc, in_=pg[:, 0:64], func=AF.Copy), reads=rpg0, writes=[rgc])
        P.op("dve", lambda e: e.tensor_scalar_mul(out=ngc, in0=pg[:, 0:64], scalar1=-1.0), reads=rpg0, writes=[rngc])
        P.op("act", lambda e: e.activation(out=gam, in_=pg[:, 0:64], func=AF.Exp), reads=rpg0, writes=[rgam])
        P.op("act", lambda e: e.activation(out=bg, in_=pg[:, 0:64], func=AF.Exp), reads=rpg0, writes=[rbg])
        P.op("dve", lambda e: e.tensor_tensor(out=bg, in0=bg, in1=beta, op=ALU.mult), reads=[rbg, rbeta], writes=[rbg])
        P.op("dve", lambda e: e.tensor_tensor(out=kds, in0=pg2[:, 128:192], in1=gc, op=ALU.subtract), reads=rpg1 + [rgc], writes=[rkds])
        P.op("act", lambda e: e.activation(out=kds, in_=kds, func=AF.Exp), reads=[rkds], writes=[rkds])
        P.op("act", lambda e: e.activation(out=gle, in_=pg2[:, 128:192], func=AF.Exp), reads=rpg1, writes=[rgle])
        P.op("dve", lambda e: e.memset(zbuf[:, 0:3], 0.0), writes=[rzp])
        if "gates" in self.taps:
            self.tap("g", g_, [128, 64], [rg_])
            self.tap("gc", gc, [128, 64], [rgc])
            self.tap("beta", beta, [128, 64], [rbeta])

        for h in range(4):
            for xi in range(3):
                sw, rw = self.acqA()
                for tt in range(4):
                    pz, rpz = self.bank()
                    self.mm8(pz, rpz, sw, rw, hrhs(tt))
                    P.op("act", lambda e, pz=pz, tt=tt: e.activation(out=zbuf[:, 3 + tt * 512:3 + (tt + 1) * 512], in_=pz[:], func=AF.Copy),
                         reads=rpz, writes=[rz[tt]])
                self.relA()
                wc = 100 + (xi * 4 + h) * 4
                for tt in range(4):
                    rd = [rz[tt], rzp, self.rC] + ([rz[tt - 1]] if tt > 0 else [])
                    a_ = acc[:, tt * 512:(tt + 1) * 512]
                    P.op("dve", lambda e, a_=a_, tt=tt, wc=wc: e.tensor_scalar_mul(out=a_, in0=zbuf[:, tt * 512 + 3:tt * 512 + 515], scalar1=self.ppcol(l, wc + 3)),
                         reads=rd, writes=[racc[tt]])
                    for j in (2, 1, 0):
                        P.op("dve", lambda e, a_=a_, tt=tt, wc=wc, j=j: e.scalar_tensor_tensor(
                            out=a_, in0=zbuf[:, tt * 512 + j:tt * 512 + j + 512], scalar=self.ppcol(l, wc + j), in1=a_, op0=ALU.mult, op1=ALU.add),
                            reads=rd + [racc[tt]], writes=[racc[tt]])
                    if xi == 2:
                        P.op("act", lambda e, a_=a_, tt=tt: e.activation(out=vT[:, tt * 512:(tt + 1) * 512], in_=a_, func=AF.Silu),
                             reads=[racc[tt]], writes=[rvT[tt]])
                        continue
                    P.op("act", lambda e, a_=a_: e.activation(out=a_, in_=a_, func=AF.Silu), reads=[racc[tt]], writes=[racc[tt]])
                    sq_, rsq_ = sqb[0]
                    P.op("act", lambda e, a_=a_, sq_=sq_: e.activation(out=sq_, in_=a_, func=AF.Square), reads=[racc[tt]], writes=[rsq_])
                    pss, rpss = self.bank()
                    P.op("pe", lambda e, pss=pss, sq_=sq_: e.matmul(pss[:], self.ones_b[:, 0, :], sq_, start=True, stop=True),
                         reads=[rsq_, self.rC], writes=rpss)
                    ri, rri = rinv[0]
                    P.op("act", lambda e, pss=pss, ri=ri: e.activation(out=ri, in_=pss[:], func=AF.Sqrt, bias=float(L2_EPS), scale=1.0),
                         reads=rpss, writes=[rri])
                    P.op("dve", lambda e, ri=ri: e.reciprocal(ri, ri), reads=[rri], writes=[rri])
                    dst, rdst = (qn, rqn) if xi == 0 else (kn, rkn)
                    scl = (128.0 ** -0.5) if xi == 0 else 1.0
                    P.op("dve", lambda e, a_=a_, ri=ri, dst=dst, tt=tt, scl=scl: e.scalar_tensor_tensor(
                        out=dst[:, tt * 512:(tt + 1) * 512], in0=a_, scalar=scl, in1=ri, op0=ALU.mult, op1=ALU.mult),
                        reads=[racc[tt], rri], writes=[rdst[tt]])
            sw, rw = self.acqA()
            for tt in range(4):
                pz, rpz = self.bank()
                self.mm8(pz, rpz, sw, rw, hrhs(tt))
                P.op("act", lambda e, pz=pz, tt=tt: e.activation(out=sgT[:, tt * 512:(tt + 1) * 512], in_=pz[:], func=AF.Silu),
                     reads=rpz, writes=[rsg[tt]])
            self.relA()
            if "gdn_qkv" in self.taps and h == 0:
                self.tap("qn0", qn, [128, S], rqn, bf=True)
                self.tap("kn0", kn, [128, S], rkn, bf=True)
                self.tap("vT0", vT, [128, S], rvT, bf=True)

            for c in range(16):
                tok = slice(c * 128, (c + 1) * 128)
                t4 = c // 4
                col = c * 4 + h
                cs1 = slice(col, col + 1)
                ptk, rptk = self.bankb()
                ptv, rptv = self.bankb()
                P.op("pe", lambda e, tok=tok, ptk=ptk: e.transpose(ptk[:, 0:128], kn[:, tok], self.ident_b[:]), reads=[rkn[t4], self.rC], writes=rptk)
                P.op("pe", lambda e, tok=tok, ptv=ptv: e.transpose(ptv[:, 0:128], vT[:, tok], self.ident_b[:]), reads=[rvT[t4], self.rC], writes=rptv)
                P.op("dve", lambda e, cs1=cs1, ptk=ptk: e.tensor_scalar_mul(out=kd, in0=ptk[:, 0:128], scalar1=kds[:, cs1]), reads=rptk + [rkds], writes=[rkd])
                P.op("act", lambda e, ptv=ptv: e.activation(out=vtm, in_=ptv[:, 0:128], func=AF.Copy), reads=rptv, writes=[rvtm])
                P.op("act", lambda e, tok=tok: e.activation(out=knf, in_=kn[:, tok], func=AF.Copy), reads=[rkn[t4]], writes=[rknf])
                pKK, rKK = self.bank()
                pQK, rQKp = self.bank()
                pGC, rGC = self.bank()
                P.op("pe", lambda e, pKK=pKK, tok=tok: e.matmul(pKK[:, 0:128], kn[:, tok], kn[:, tok], start=True, stop=True), reads=[rkn[t4]], writes=rKK)
                P.op("pe", lambda e, pQK=pQK, tok=tok: e.matmul(pQK[:, 0:128], kn[:, tok], qn[:, tok], start=True, stop=True), reads=[rkn[t4], rqn[t4]], writes=rQKp)
                P.op("dve", lambda e, cs1=cs1: e.tensor_scalar_mul(out=dgc, in0=self.ident_f, scalar1=gc[:, cs1]), reads=[rgc, self.rC], writes=[rdgc])
                P.op("pe", lambda e, pGC=pGC: e.matmul(pGC[:, 0:128], self.ones_f[:], dgc, start=True, stop=True), reads=[rdgc, self.rC], writes=rGC)
                gcr = pGC[:, 0:128]
                P.op("dve", lambda e, gcr=gcr: e.scalar_tensor_tensor(out=tl, in0=gcr, scalar=-1.0, in1=self.nmls, op0=ALU.mult, op1=ALU.add),
                     reads=rGC + [self.rC], writes=[rtl])
                P.op("act", lambda e, cs1=cs1: e.activation(out=El, in_=tl, func=AF.Exp, bias=gc[:, cs1], scale=1.0), reads=[rtl, rgc], writes=[rEl])
                P.op("dve", lambda e, gcr=gcr: e.tensor_tensor(out=tu, in0=gcr, in1=self.nmui, op=ALU.add), reads=rGC + [self.rC], writes=[rtu])
                P.op("act", lambda e, cs1=cs1: e.activation(out=Eu, in_=tu, func=AF.Exp, bias=ngc[:, cs1], scale=1.0), reads=[rtu, rngc], writes=[rEu])
                P.op("act", lambda e, gcr=gcr: e.activation(out=gamr, in_=gcr, func=AF.Exp), reads=rGC, writes=[rgamr])
                A0, rA0 = Ab[0]
                B0, rB0 = Bb[0]
                P.op("dve", lambda e, pKK=pKK, cs1=cs1, A0=A0: e.scalar_tensor_tensor(
                    out=A0, in0=pKK[:, 0:128], scalar=nbeta[:, cs1], in1=El, op0=ALU.mult, op1=ALU.mult),
                    reads=rKK + [rnbeta, rEl], writes=[rA0])
                pta, rpta = self.bank()
                P.op("pe", lambda e, A0=A0, pta=pta: e.transpose(pta[:, 0:128], A0, self.ident_f), reads=[rA0, self.rC], writes=rpta)
                P.op("act", lambda e, B0=B0, pta=pta: e.activation(out=B0, in_=pta[:, 0:128], func=AF.Copy), reads=rpta, writes=[rB0])
                P.op("dve", lambda e, pQK=pQK: e.tensor_tensor(out=intraT, in0=pQK[:, 0:128], in1=Eu, op=ALU.mult), reads=rQKp + [rEu], writes=[rintra])
                P.op("dve", lambda e, tok=tok: e.tensor_tensor(out=qg, in0=qn[:, tok], in1=gamr, op=ALU.mult), reads=[rqn[t4], rgamr], writes=[rqg])
                TT, rTT = TTb[0]
                P.op("dve", lambda e, TT=TT, B0=B0: e.tensor_tensor(out=TT, in0=self.ident_f, in1=B0, op=ALU.add), reads=[rB0, self.rC], writes=[rTT])
                cur = 0
                for k in range(1, 7):
                    Ap, rAp = Ab[cur]
                    Bp, rBp = Bb[cur]
                    An, rAn = Ab[1 - cur]
                    Bn, rBn = Bb[1 - cur]
                    TTp, rTTp = TTb[cur]
                    TTn, rTTn = TTb[1 - cur]
                    pa2, rpa2 = self.bank()
                    P.op("pe", lambda e, pa2=pa2, Ap=Ap, Bp=Bp: e.matmul(pa2[:, 0:128], Bp, Ap, start=True, stop=True), reads=[rAp, rBp], writes=rpa2)
                    if k < 6:
                        pb2, rpb2 = self.bank()
                        P.op("pe", lambda e, pb2=pb2, Ap=Ap, Bp=Bp: e.matmul(pb2[:, 0:128], Ap, Bp, start=True, stop=True), reads=[rAp, rBp], writes=rpb2)
                    P.op("act", lambda e, pa2=pa2, An=An: e.activation(out=An, in_=pa2[:, 0:128], func=AF.Copy), reads=rpa2, writes=[rAn])
                    if k < 6:
                        P.op("dve", lambda e, pb2=pb2, Bn=Bn: e.tensor_copy(Bn, pb2[:, 0:128]), reads=rpb2, writes=[rBn])
                    pt2, rpt2 = self.bank()
                    P.op("pe", lambda e, pt2=pt2, An=An, TTp=TTp: e.matmul(pt2[:, 0:128], An, TTp, start=True, stop=True), reads=[rAn, rTTp], writes=rpt2)
                    P.op("dve", lambda e, pt2=pt2, TTp=TTp, TTn=TTn: e.tensor_tensor(out=TTn, in0=TTp, in1=pt2[:, 0:128], op=ALU.add),
                         reads=rpt2 + [rTTp], writes=[rTTn])
                    cur = 1 - cur
                TT, rTT = TTb[cur]
                if c > 0:
                    pks, rks = self.bank()
                    P.op("pe", lambda e, pks=pks: e.matmul(pks[:, 0:128], knf, Sf, start=True, stop=True), reads=[rknf, rSf], writes=rks)
                    P.op("dve", lambda e, pks=pks, cs1=cs1: e.scalar_tensor_tensor(
                        out=rr_, in0=pks[:, 0:128], scalar=gam[:, cs1], in1=vtm, op0=ALU.mult, op1=ALU.subtract),
                        reads=rks + [rgam, rvtm], writes=[rrr])
                    P.op("dve", lambda e, cs1=cs1: e.tensor_scalar_mul(out=rr_, in0=rr_, scalar1=nbeta[:, cs1]), reads=[rrr, rnbeta], writes=[rrr])
                else:
                    P.op("dve", lambda e, cs1=cs1: e.tensor_scalar_mul(out=rr_, in0=vtm, scalar1=beta[:, cs1]), reads=[rvtm, rbeta], writes=[rrr])
                pWv, rWv = self.bank()
                pWo, rWo = self.bank()
                pWs, rWs = self.bank()
                P.op("pe", lambda e, pWv=pWv, TT=TT: e.matmul(pWv[:, 0:128], TT, rr_, start=True, stop=True), reads=[rTT, rrr], writes=rWv)
                P.op("act", lambda e, pWv=pWv: e.activation(out=vnew, in_=pWv[:, 0:128], func=AF.Copy), reads=rWv, writes=[rvnew])
                if c > 0:
                    P.op("pe", lambda e, pWo=pWo: e.matmul(pWo[:, 0:128], qg, Sb, start=True, stop=False), reads=[rqg, rSb], writes=rWo)
                P.op("pe", lambda e, pWo=pWo, c=c: e.matmul(pWo[:, 0:128], intraT, vnew, start=(c == 0), stop=True), reads=[rintra, rvnew], writes=rWo)
                P.op("pe", lambda e, pWs=pWs: e.matmul(pWs[:, 0:128], kd, vnew, start=True, stop=True), reads=[rkd, rvnew], writes=rWs)
                if c == 0:
                    P.op("dve", lambda e, pWs=pWs: e.tensor_copy(Sf, pWs[:, 0:128]), reads=rWs, writes=[rSf])
                else:
                    P.op("dve", lambda e, pWs=pWs, cs1=cs1: e.scalar_tensor_tensor(
                        out=Sf, in0=Sf, scalar=gle[:, cs1], in1=pWs[:, 0:128], op0=ALU.mult, op1=ALU.add),
                        reads=rWs + [rSf, rgle], writes=[rSf])
                P.op("act", lambda e: e.activation(out=Sb, in_=Sf, func=AF.Copy), reads=[rSf], writes=[rSb])
                po_ = pWo[:, 0:128]
                P.op("dve", lambda e: e.memset(ss[:, 0:1], 0.0), writes=[rss])
                P.op("act", lambda e, po_=po_: e.activation(out=junk, in_=po_, func=AF.Square, accum_out=ss[:, 0:1]), reads=rWo + [rss], writes=[rjunk, rss])
                P.op("act", lambda e: e.activation(out=ss[:, 1:2], in_=ss[:, 0:1], func=AF.Sqrt, bias=float(RMS_EPS), scale=1.0 / 128), reads=[rss], writes=[rss])
                P.op("dve", lambda e: e.reciprocal(ss[:, 1:2], ss[:, 1:2]), reads=[rss], writes=[rss])
                P.op("dve", lambda e, po_=po_: e.tensor_scalar_mul(out=onb, in0=po_, scalar1=ss[:, 1:2]), reads=rWo + [rss], writes=[ronb])
                pto, rpto = self.bankb()
                P.op("pe", lambda e, pto=pto: e.transpose(pto[:, 0:128], onb, self.ident_b[:]), reads=[ronb, self.rC], writes=rpto)
                P.op("dve", lambda e, tok=tok, h=h, pto=pto: e.scalar_tensor_tensor(
                    out=mixT[:, 4 + h, tok], in0=pto[:, 0:128], scalar=self.ppcol(l, 148), in1=sgT[:, tok], op0=ALU.mult, op1=ALU.mult),
                    reads=rpto + [rsg[t4], self.rC], writes=[rM[4 + h][t4]])

    def ffn(self, l):
        P = self.P
        P.barrier()
        hb = self.view(0, 4096, BF16, (KC, 1024))
        act = self.view(4096, 11264, BF16, (FC, 1024))
        f2 = self.view(15360, 8192, F32, (KC, 1024))
        so = 23552
        sq = [(self.view(so + i * 256, 256, BF16), Res("fsq%d" % i)) for i in range(2)]
        rstdb = [(self.view(so + 512 + i * 512, 512), Res("frstd%d" % i)) for i in range(2)]
        tmpb = [(self.view(so + 1536 + i * 512, 512), Res("ftmp%d" % i)) for i in range(2)]
        sgl = [(self.view(so + 2560 + i * 512, 512), Res("sgl%d" % i)) for i in range(2)]
        assert so + 3584 <= self.ARW
        rHb = [[Res("hb%d_%d" % (c, t)) for t in range(2)] for c in range(KC)]
        rAct = [[Res("a%d_%d" % (j, t)) for t in range(2)] for j in range(FC)]
        rF2 = [[Res("g%d_%d" % (c, t)) for t in range(2)] for c in range(KC)]
        for half in range(2):
            tts = [half * 2, half * 2 + 1]
            self.prenorm(l, 16, hb, rHb, tts, sq, rstdb)
            for j in range(FC):
                sg, rg = self.acqA()
                su, ru = self.acqA()
                for t2 in range(2):
                    pg, rpg = self.bank()
                    pu, rpu = self.bank()
                    rhs = lambda k, t2=t2: (hb[:, k, t2 * 512:(t2 + 1) * 512], rHb[k][t2])
                    self.mm8(pg, rpg, sg, rg, rhs)
                    self.mm8(pu, rpu, su, ru, rhs)
                    s_, rs_ = sgl[t2]
                    P.op("act", lambda e, pg=pg, s_=s_: e.activation(out=s_, in_=pg[:], func=AF.Silu), reads=rpg, writes=[rs_])
                    P.op("dve", lambda e, pu=pu, s_=s_, j=j, t2=t2: e.tensor_tensor(
                        out=act[:, j, t2 * 512:(t2 + 1) * 512], in0=pu[:], in1=s_, op=ALU.mult),
                        reads=rpu + [rs_], writes=[rAct[j][t2]])
                self.relA(2)
            for oc in range(KC):
                sd, rd = self.acqB()
                for t2 in range(2):
                    pf, rpf = self.bank()
                    self.mm8(pf, rpf, sd, rd, lambda k, t2=t2: (act[:, k, t2 * 512:(t2 + 1) * 512], rAct[k][t2]), nk=FC)
                    fa = f2[:, oc, t2 * 512:(t2 + 1) * 512]
                    if (oc + t2) % 2 == 0:
                        P.op("act", lambda e, pf=pf, fa=fa: e.activation(out=fa, in_=pf[:], func=AF.Copy), reads=rpf, writes=[rF2[oc][t2]])
                    else:
                        P.op("dve", lambda e, pf=pf, fa=fa: e.tensor_copy(fa, pf[:]), reads=rpf, writes=[rF2[oc][t2]])
                self.relB()
            self.postnorm_add(l, 24, lambda c, li: (f2[:, c, li * 512:(li + 1) * 512], rF2[c][li]), tts, sq, rstdb, tmpb)

    def build(self, do_mixer=True, do_ffn=True):
        with contextlib.ExitStack() as st:
            self.alloc(st)
            self.weight_lists()
            self.setup()
            for s in range(self.nseq):
                self.load_x(s)
                for l in range(self.nl):
                    self.mixer(l)
                    if self.stop is None or self.stop == "ffn":
                        self.ffn(l)
                self.store_x(s)
            fw = [k for k in self.P.dma_sems if k not in ("c", "cs") and not (isinstance(k, tuple) and k[0] == "x")]
            self.P.emit(final_waits=fw)
        return self.nc


def _pack_inputs(inp):
    f = lambda a: np.ascontiguousarray(np.asarray(a, dtype=np.float32))
    cols = _win_cols()
    w_in = f(inp["w_in"])
    win = np.stack([_chunk_w(w_in[l], cols) for l in range(L)]).reshape(L, NWIN, 128, KC * 128)
    c128 = lambda n: [np.arange(i * 128, (i + 1) * 128) for i in range(n // 128)]
    wout = np.stack([_chunk_w(f(inp["w_out"])[l], c128(D)) for l in range(L)]).reshape(L, KC, 128, KC * 128)
    wg = np.stack([_chunk_w(f(inp["w_gate"])[l], c128(FF)) for l in range(L)]).reshape(L, FC, 128, KC * 128)
    wu = np.stack([_chunk_w(f(inp["w_up"])[l], c128(FF)) for l in range(L)]).reshape(L, FC, 128, KC * 128)
    wd = np.stack([_chunk_w(f(inp["w_down"])[l], c128(D)) for l in range(L)]).reshape(L, KC, 128, FC * 128)
    pp = np.zeros((128, L * PP_PER_L), np.float32)
    gdnc = np.zeros((1, L * 128), np.float32)
    for l in range(L):
        b = l * PP_PER_L
        for off, key in ((0, "attn_pre_norm"), (8, "attn_post_norm"), (16, "ffn_pre_norm"), (24, "ffn_post_norm")):
            pp[:, b + off:b + off + 8] = f(inp[key])[l].reshape(8, 128).T
        cw = f(inp["conv_dw"])[l]
        for cc in range(2):
            pp[:, b + 32 + cc * 31:b + 32 + (cc + 1) * 31] = cw[:, cc * 128:(cc + 1) * 128].T
        pp[:, b + 94:b + 96] = f(inp["conv_dw_bias"])[l].reshape(2, 128).T
        pp[:, b + 96:b + 98] = f(inp["conv_norm_gain"])[l].reshape(2, 128).T
        pp[:, b + 98:b + 100] = f(inp["conv_norm_bias"])[l].reshape(2, 128).T
        gs = f(inp["gdn_short_conv"])[l]
        for xi in range(3):
            for h in range(4):
                ch = xi * 512 + h * 128
                pp[:, b + 100 + (xi * 4 + h) * 4:b + 100 + (xi * 4 + h) * 4 + 4] = gs[:, ch:ch + 128].T
        pp[:, b + 148] = f(inp["gdn_out_norm"])[l]
        gdnc[0, l * 128:l * 128 + 64] = np.tile(f(inp["gdn_a_log"])[l], 16)
        gdnc[0, l * 128 + 64:l * 128 + 128] = np.tile(f(inp["gdn_dt_bias"])[l], 16)
    cs, ms, cmat = _host_consts()
    return dict(win=win, wout=wout, wgate=wg, wup=wu, wdown=wd, pp=pp, gdnc=gdnc, cs=cs, ms=ms, cmat=cmat)


def kernel(**inputs):
    x = np.asarray(inputs["x"], dtype=np.float32)
    shared = _pack_inputs(inputs)
    b = Builder(SEQ_PER_CORE, L)
    nc = b.build()
    in_maps = []
    for c in range(NCORE):
        xs = x[c * SEQ_PER_CORE:(c + 1) * SEQ_PER_CORE]
        m = dict(shared)
        m["xT"] = np.ascontiguousarray(xs.transpose(0, 2, 1))
        in_maps.append(m)
    res = run_bass_kernel_spmd(nc, in_maps, core_ids=list(range(NCORE)))
    out = np.empty_like(x)
    for c in range(NCORE):
        out[c * SEQ_PER_CORE:(c + 1) * SEQ_PER_CORE] = res.results[c]["outT"].transpose(0, 2, 1)
    return out
```
